# Optimizing a Trainium2 kernel written in Bass

```python
import math
import jax, jax.numpy as jnp
from jax import lax
import numpy as np


D_MODEL = 1024
BATCH = 4
SEQ = 8192
DEPTH = 4

RMS_EPS = 1e-6
Q_BLOCK = 128
NEG_INF = -1e30

T5_BUCKETS = 32
T5_MAX_EXACT = T5_BUCKETS // 2
T5_MAX_DISTANCE = 1024

DIFF_HEADS = 4
DIFF_HEAD_DIM = 64
DIFF_QK_WIDTH = DIFF_HEADS * 2 * DIFF_HEAD_DIM
DIFF_V_WIDTH = DIFF_HEADS * 2 * DIFF_HEAD_DIM

MOBA_HEADS = 4
MOBA_HEAD_DIM = 128
MOBA_BLOCK = 256
MOBA_TOPK = 3
MOBA_Q_CHUNK = 32
MOBA_WIDTH = MOBA_HEADS * MOBA_HEAD_DIM

MLA_HEADS = 8
MLA_Q_RANK = 256
MLA_KV_RANK = 128
MLA_NOPE_DIM = 64
MLA_ROPE_DIM = 32
MLA_V_DIM = 64
MLA_WIDTH = MLA_HEADS * MLA_V_DIM
ROPE_THETA = 10000.0

N_BRANCH = 3
BRANCH_WIDTH = 512
N_BIAS_HEADS = DIFF_HEADS + MOBA_HEADS
D_FF = 4 * D_MODEL

IN_SPLITS = (DIFF_QK_WIDTH, DIFF_QK_WIDTH, DIFF_V_WIDTH,
             MOBA_WIDTH, MOBA_WIDTH, MOBA_WIDTH,
             MLA_Q_RANK, MLA_KV_RANK, MLA_ROPE_DIM,
             N_BRANCH * D_MODEL)
IN_OFFSETS = tuple(sum(IN_SPLITS[:i + 1]) for i in range(len(IN_SPLITS) - 1))
D_IN = sum(IN_SPLITS)

kernel_name = 'hybrid_diff_moba_mla_gated_trunk'


def _rms_norm(x, w):
    xf = x.astype(jnp.float32)
    y = xf * lax.rsqrt(jnp.mean(xf * xf, axis=-1, keepdims=True) + RMS_EPS)
    return (y * w.astype(jnp.float32)).astype(x.dtype)


def _t5_bucket(rel):
    n = jnp.maximum(rel, 0)
    nf = jnp.maximum(n, 1).astype(jnp.float32)
    large = T5_MAX_EXACT + (jnp.log(nf / T5_MAX_EXACT)
                            / math.log(T5_MAX_DISTANCE / T5_MAX_EXACT)
                            * (T5_BUCKETS - T5_MAX_EXACT)).astype(jnp.int32)
    large = jnp.minimum(large, T5_BUCKETS - 1)
    return jnp.where(n < T5_MAX_EXACT, n, large)


def _rope(x, positions):
    d = x.shape[-1]
    half = d // 2
    inv_freq = ROPE_THETA ** (-jnp.arange(half, dtype=jnp.float32) * 2.0 / d)
    ang = positions.astype(jnp.float32)[:, :, None, None] * inv_freq
    cos, sin = jnp.cos(ang), jnp.sin(ang)
    xf = x.astype(jnp.float32)
    x1, x2 = xf[..., :half], xf[..., half:]
    return jnp.concatenate([x1 * cos - x2 * sin, x2 * cos + x1 * sin], axis=-1).astype(x.dtype)


def _diff_attention(q1, q2, k1, k2, v, lam, bias_table):
    B, S, H, dh = q1.shape
    scale = 1.0 / math.sqrt(dh)
    k_pos = jnp.arange(S)
    table = bias_table.astype(jnp.float32)

    def block(c):
        start = c * Q_BLOCK
        q_pos = start + jnp.arange(Q_BLOCK)
        rel = q_pos[:, None] - k_pos[None, :]
        bias = jnp.transpose(table[_t5_bucket(rel)], (2, 0, 1))
        visible = rel >= 0

        def probs(q, k):
            qb = lax.dynamic_slice_in_dim(q, start, Q_BLOCK, axis=1)
            s = jnp.einsum('bqhd,bkhd->bhqk', qb, k).astype(jnp.float32) * scale + bias
            return jax.nn.softmax(jnp.where(visible, s, NEG_INF), axis=-1)

        w = probs(q1, k1) - lam * probs(q2, k2)
        return jnp.einsum('bhqk,bkhd->bqhd', w.astype(v.dtype), v)

    out = lax.map(block, jnp.arange(S // Q_BLOCK))
    return jnp.transpose(out, (1, 0, 2, 3, 4)).reshape(B, S, H, v.shape[-1])


def _moba_attention(q, k, v, bias_table):
    B, S, H, d = q.shape
    scale = 1.0 / math.sqrt(d)
    n_blk = -(-S // MOBA_BLOCK)
    s_pad = n_blk * MOBA_BLOCK
    topk = min(MOBA_TOPK, n_blk)
    pad = ((0, 0), (0, s_pad - S), (0, 0), (0, 0))
    kbh = jnp.transpose(jnp.pad(k, pad).reshape(B, n_blk, MOBA_BLOCK, H, d), (0, 3, 1, 2, 4))
    vbh = jnp.transpose(jnp.pad(v, pad).reshape(B, n_blk, MOBA_BLOCK, H, d), (0, 3, 1, 2, 4))
    k_mean = jnp.mean(kbh.astype(jnp.float32), axis=3)
    blk_ids = jnp.arange(n_blk)
    offs = jnp.arange(MOBA_BLOCK)
    table = bias_table.astype(jnp.float32)
    table_t = table.T
    bi = jnp.arange(B)[:, None, None, None]
    hi = jnp.arange(H)[None, :, None, None]
    hi5 = jnp.arange(H)[None, :, None, None, None]

    def chunk(c):
        start = c * MOBA_Q_CHUNK
        own = start // MOBA_BLOCK
        q_pos = start + jnp.arange(MOBA_Q_CHUNK)
        qbt = jnp.transpose(lax.dynamic_slice_in_dim(q, start, MOBA_Q_CHUNK, axis=1), (0, 2, 1, 3))
        gate = jnp.einsum('bhqd,bhnd->bhqn', qbt.astype(jnp.float32), k_mean)
        gate = jnp.where(blk_ids < own, gate, NEG_INF)
        _, idx = lax.top_k(gate, topk)
        valid = idx < own
        k_sel = kbh[bi, hi, idx]
        v_sel = vbh[bi, hi, idx]
        s_sel = jnp.einsum('bhqd,bhqnld->bhqnl', qbt, k_sel).astype(jnp.float32) * scale
        sel_pos = idx[..., None] * MOBA_BLOCK + offs
        rel_sel = q_pos[None, None, :, None, None] - sel_pos
        s_sel = jnp.where(valid[..., None], s_sel + table_t[hi5, _t5_bucket(rel_sel)], NEG_INF)
        k_own = lax.dynamic_index_in_dim(kbh, own, axis=2, keepdims=False)
        v_own = lax.dynamic_index_in_dim(vbh, own, axis=2, keepdims=False)
        rel_own = q_pos[:, None] - (own * MOBA_BLOCK + offs)[None, :]
        bias_own = jnp.transpose(table[_t5_bucket(rel_own)], (2, 0, 1))
        s_own = jnp.einsum('bhqd,bhld->bhql', qbt, k_own).astype(jnp.float32) * scale + bias_own
        s_own = jnp.where(rel_own >= 0, s_own, NEG_INF)
        n_sel = topk * MOBA_BLOCK
        s = jnp.concatenate([s_sel.reshape(B, H, MOBA_Q_CHUNK, n_sel), s_own], axis=-1)
        p = jax.nn.softmax(s, axis=-1).astype(v.dtype)
        p_sel = p[..., :n_sel].reshape(B, H, MOBA_Q_CHUNK, topk, MOBA_BLOCK)
        p_own = p[..., n_sel:]
        return (jnp.einsum('bhqnl,bhqnld->bqhd', p_sel, v_sel)
                + jnp.einsum('bhql,bhld->bqhd', p_own, v_own))

    out = lax.map(chunk, jnp.arange(S // MOBA_Q_CHUNK))
    return jnp.transpose(out, (1, 0, 2, 3, 4)).reshape(B, S, H, d)


def _dense_causal_attention(q, k, v, scale):
    B, S, H, _ = q.shape
    k_pos = jnp.arange(S)

    def block(c):
        start = c * Q_BLOCK
        qb = lax.dynamic_slice_in_dim(q, start, Q_BLOCK, axis=1)
        s = jnp.einsum('bqhd,bkhd->bhqk', qb, k).astype(jnp.float32) * scale
        rel = (start + jnp.arange(Q_BLOCK))[:, None] - k_pos[None, :]
        p = jax.nn.softmax(jnp.where(rel >= 0, s, NEG_INF), axis=-1).astype(v.dtype)
        return jnp.einsum('bhqk,bkhd->bqhd', p, v)

    out = lax.map(block, jnp.arange(S // Q_BLOCK))
    return jnp.transpose(out, (1, 0, 2, 3, 4)).reshape(B, S, H, v.shape[-1])


def setup_inputs(seed: int = 0) -> dict:
    key = jax.random.key(seed)
    ks = jax.random.split(key, 20)

    def nrm(k, shape, scale):
        return jax.random.normal(k, shape, jnp.float32) * scale

    x = nrm(ks[0], (BATCH, SEQ, D_MODEL), 1.0)
    offsets = jax.random.randint(ks[1], (BATCH, 1), 0, 4096, dtype=jnp.int32)
    positions = offsets + jnp.arange(SEQ, dtype=jnp.int32)[None, :]
    return {
        'x': x,
        'positions': positions,
        'rel_bias': nrm(ks[2], (T5_BUCKETS, N_BIAS_HEADS), 0.5),
        'norm_mix_pre': 1.0 + nrm(ks[3], (DEPTH, D_MODEL), 0.05),
        'norm_mix_post': 1.0 + nrm(ks[4], (DEPTH, D_MODEL), 0.05),
        'norm_mlp_pre': 1.0 + nrm(ks[5], (DEPTH, D_MODEL), 0.05),
        'norm_mlp_post': 1.0 + nrm(ks[6], (DEPTH, D_MODEL), 0.05),
        'w_in': nrm(ks[7], (DEPTH, D_MODEL, D_IN), D_MODEL ** -0.5),
        'diff_lambda': nrm(ks[8], (DEPTH, 4, DIFF_HEAD_DIM), 0.1),
        'diff_subln': 1.0 + nrm(ks[9], (DEPTH, 2 * DIFF_HEAD_DIM), 0.05),
        'mla_q_norm': 1.0 + nrm(ks[10], (DEPTH, MLA_Q_RANK), 0.05),
        'mla_w_uq': nrm(ks[11], (DEPTH, MLA_Q_RANK, MLA_HEADS * (MLA_NOPE_DIM + MLA_ROPE_DIM)), MLA_Q_RANK ** -0.5),
        'mla_kv_norm': 1.0 + nrm(ks[12], (DEPTH, MLA_KV_RANK), 0.05),
        'mla_w_ukv': nrm(ks[13], (DEPTH, MLA_KV_RANK, MLA_HEADS * (MLA_NOPE_DIM + MLA_V_DIM)), MLA_KV_RANK ** -0.5),
        'w_branch': nrm(ks[14], (DEPTH, N_BRANCH, BRANCH_WIDTH, D_MODEL), BRANCH_WIDTH ** -0.5),
        'w_out': nrm(ks[15], (DEPTH, D_MODEL, D_MODEL), D_MODEL ** -0.5),
        'w_up': nrm(ks[16], (DEPTH, D_MODEL, D_FF), D_MODEL ** -0.5),
        'w_down': nrm(ks[17], (DEPTH, D_FF, D_MODEL), D_FF ** -0.5),
    }


def reference(x, positions, rel_bias, norm_mix_pre, norm_mix_post, norm_mlp_pre, norm_mlp_post,
              w_in, diff_lambda, diff_subln, mla_q_norm, mla_w_uq, mla_kv_norm, mla_w_ukv,
              w_branch, w_out, w_up, w_down):
    B, S, _ = x.shape
    for l in range(DEPTH):
        lam_init = 0.8 - 0.6 * math.exp(-0.3 * l)
        h = _rms_norm(x, norm_mix_pre[l])
        proj = jnp.einsum('bsd,de->bse', h, w_in[l])
        dq, dk, dv, mq, mk, mv, c_q, c_kv, k_pe, g = jnp.split(proj, IN_OFFSETS, axis=-1)

        dq = dq.reshape(B, S, DIFF_HEADS, 2, DIFF_HEAD_DIM)
        dk = dk.reshape(B, S, DIFF_HEADS, 2, DIFF_HEAD_DIM)
        dv = dv.reshape(B, S, DIFF_HEADS, 2 * DIFF_HEAD_DIM)
        lam_vecs = diff_lambda[l].astype(jnp.float32)
        lam = (jnp.exp(jnp.sum(lam_vecs[0] * lam_vecs[1]))
               - jnp.exp(jnp.sum(lam_vecs[2] * lam_vecs[3])) + lam_init)
        oa = _diff_attention(dq[..., 0, :], dq[..., 1, :], dk[..., 0, :], dk[..., 1, :], dv,
                             lam, rel_bias[:, :DIFF_HEADS])
        oa = (_rms_norm(oa, diff_subln[l]) * (1.0 - lam_init)).reshape(B, S, DIFF_V_WIDTH)

        ob = _moba_attention(mq.reshape(B, S, MOBA_HEADS, MOBA_HEAD_DIM),
                             mk.reshape(B, S, MOBA_HEADS, MOBA_HEAD_DIM),
                             mv.reshape(B, S, MOBA_HEADS, MOBA_HEAD_DIM),
                             rel_bias[:, DIFF_HEADS:]).reshape(B, S, MOBA_WIDTH)

        q = jnp.einsum('bsr,re->bse', _rms_norm(c_q, mla_q_norm[l]), mla_w_uq[l])
        q = q.reshape(B, S, MLA_HEADS, MLA_NOPE_DIM + MLA_ROPE_DIM)
        q = jnp.concatenate([q[..., :MLA_NOPE_DIM], _rope(q[..., MLA_NOPE_DIM:], positions)], axis=-1)
        kv = jnp.einsum('bsr,re->bse', _rms_norm(c_kv, mla_kv_norm[l]), mla_w_ukv[l])
        kv = kv.reshape(B, S, MLA_HEADS, MLA_NOPE_DIM + MLA_V_DIM)
        k_rope = jnp.broadcast_to(_rope(k_pe[:, :, None, :], positions), (B, S, MLA_HEADS, MLA_ROPE_DIM))
        k = jnp.concatenate([kv[..., :MLA_NOPE_DIM], k_rope], axis=-1)
        oc = _dense_causal_attention(q, k, kv[..., MLA_NOPE_DIM:],
                                     1.0 / math.sqrt(MLA_NOPE_DIM + MLA_ROPE_DIM)).reshape(B, S, MLA_WIDTH)

        branches = jnp.stack([oa, ob, oc], axis=2)
        br = jnp.einsum('bsgc,gcd->bsgd', branches, w_branch[l])
        gates = jax.nn.sigmoid(g.reshape(B, S, N_BRANCH, D_MODEL))
        mixed = jnp.einsum('bsd,de->bse', jnp.sum(gates * br, axis=2), w_out[l])
        x = x + _rms_norm(mixed, norm_mix_post[l])

        h = _rms_norm(x, norm_mlp_pre[l])
        u = jnp.square(jax.nn.relu(jnp.einsum('bsd,df->bsf', h, w_up[l])))
        x = x + _rms_norm(jnp.einsum('bsf,fd->bsd', u, w_down[l]), norm_mlp_post[l])
    return x
```

```python
import math
from contextlib import ExitStack

import numpy as np
import ml_dtypes

import concourse.bass as bass
import concourse.mybir as mybir
from concourse.bass_utils import run_bass_kernel_spmd

F32 = mybir.dt.float32
BF16 = mybir.dt.bfloat16
I32 = mybir.dt.int32
AF = mybir.ActivationFunctionType
ALU = mybir.AluOpType
AX = mybir.AxisListType

D = 1024
DIN = 6560
DFF = 4096
EPS = 1e-6
NEG = -30000.0
SW = 1920
C_DQ, C_DK, C_DV, C_MQ, C_MK, C_MV, C_CQ, C_CKV, C_KPE, C_G = (
    0, 512, 1024, 1536, 2048, 2560, 3072, 3328, 3456, 3488)
INVF = [1.0, 0.5623413324356079, 0.3162277638912201, 0.17782793939113617,
        0.10000000149011612, 0.05623413249850273, 0.03162277489900589,
        0.017782794311642647, 0.009999999776482582, 0.005623413249850273,
        0.003162277629598975, 0.0017782794311642647, 0.0010000000474974513,
        0.000562341301701963, 0.0003162277571391314, 0.00017782794020604342]


class _Rec:
    def __init__(self):
        self.calls = []

    def __getattr__(self, name):
        def f(*a, **k):
            self.calls.append((name, a, k))
            return None
        return f


class Sched:
    ENG = ("sp", "act", "pe", "dve", "pool")

    def __init__(self, nc):
        self.nc = nc
        self.ops = []

    def add(self, eng, fn, r=(), w=(), dma=None):
        rec = _Rec()
        fn(rec)
        assert rec.calls, "op emitted nothing"
        if dma is not None:
            assert len(rec.calls) == 1
        if eng != "pe" and len(rec.calls) > 1:
            for c in rec.calls:
                self.ops.append([eng, [c], tuple(r), tuple(w), dma])
            return
        self.ops.append([eng, rec.calls, tuple(r), tuple(w), dma])

    def barrier(self):
        self.ops.append(["barrier"])

    def emit(self):
        import os
        if os.environ.get("KTRUNC"):
            self.ops = self.ops[:int(os.environ["KTRUNC"])]
        nc, ops = self.nc, self.ops
        n = len(ops)
        lastw, rd_eng, rd_dma = {}, {}, {}
        deps = [None] * n
        needs = [False] * n
        for i, op in enumerate(ops):
            if op[0] == "barrier":
                continue
            eng, fn, r, w, dma = op
            d = set()
            for k in r:
                if k in lastw:
                    d.add(lastw[k])
            for k in w:
                if k in lastw:
                    d.add(lastw[k])
                d.update(rd_eng.get(k, {}).values())
                d.update(rd_dma.get(k, ()))
            d.discard(i)
            for k in r:
                if dma is None:
                    rd_eng.setdefault(k, {})[eng] = i
                else:
                    rd_dma.setdefault(k, []).append(i)
            for k in w:
                lastw[k] = i
                rd_eng[k] = {}
                rd_dma[k] = []
            dd = []
            for j in sorted(d):
                oj = ops[j]
                if oj[4] is None and dma is None and oj[0] == eng == "pe":
                    continue
                dd.append(j)
                if oj[4] is None:
                    needs[j] = True
            deps[i] = dd
        last_on = {}
        for i, op in enumerate(ops):
            if op[0] == "barrier":
                for j in last_on.values():
                    needs[j] = True
            elif op[4] is None:
                last_on[op[0]] = i
        for j in last_on.values():
            needs[j] = True
        RING = {"sp": 6, "pool": 5}
        cnt = {e: 0 for e in self.ENG}
        dcnt = {}
        ndma = {}
        ring_last = {}
        val = [None] * n
        for i, op in enumerate(ops):
            if op[0] == "barrier":
                op.append((dict(cnt), dict(dcnt)))
                continue
            if op[4] is not None:
                q = op[0]
                k = (q, ndma.get(q, 0) % RING[q])
                ndma[q] = ndma.get(q, 0) + 1
                if k in ring_last:
                    deps[i] = list(deps[i]) + [ring_last[k]]
                ring_last[k] = i
                dcnt[k] = dcnt.get(k, 0) + 16
                val[i] = (("dma", k), dcnt[k])
            elif needs[i]:
                cnt[op[0]] += 1
                val[i] = (("eng", op[0]), cnt[op[0]])
        final = (dict(cnt), dict(dcnt))
        keys = [("eng", e) for e in self.ENG] + [("dma", k) for k in dcnt]
        assert len(keys) <= 16, len(keys)
        with ExitStack() as st:
            sems = {}
            for idx, k in enumerate(keys):
                sems[k] = st.enter_context(nc.semaphore("s%d" % idx))
            with nc.Block() as block:
                def mk(E):
                    def body(e):
                        waited = {}

                        def wait(sk, v):
                            if v > 0 and waited.get(sk, 0) < v:
                                e.wait_ge(sems[sk], v)
                                waited[sk] = v

                        def wait_all(state):
                            cn, dc = state
                            for en, v in cn.items():
                                wait(("eng", en), v)
                            for k, v in dc.items():
                                wait(("dma", k), v)

                        for i, op in enumerate(ops):
                            if op[0] == "barrier":
                                wait_all(op[-1])
                                continue
                            if op[0] != E:
                                continue
                            for j in deps[i]:
                                wait(*val[j])
                            ins = None
                            for (nm, a, k) in op[1]:
                                ins = getattr(e, nm)(*a, **k)
                            if val[i] is not None:
                                ins.then_inc(sems[val[i][0]], 16 if val[i][0][0] == "dma" else 1)
                        wait_all(final)
                    return body
                block.sync(mk("sp"))
                block.scalar(mk("act"))
                block.tensor(mk("pe"))
                block.vector(mk("dve"))
                block.gpsimd(mk("pool"))


def build(S, L, dbg=False, upto=None):
    NT = S // 128
    NQ = S // 512
    NB = S // 256
    nc = bass.Bass("TRN2", target_bir_lowering=False)
    sc = Sched(nc)

    def din(name, shape, dt=F32):
        return nc.dram_tensor(name, list(shape), dt, kind="ExternalInput").ap()

    def dscr(name, shape, dt):
        if dbg:
            return nc.dram_tensor(name, list(shape), dt, kind="ExternalOutput").ap()
        return nc.dram_tensor(name, list(shape), dt).ap()

    x_in = din("x", [S, D])
    pos_in = din("pos", [128, NT], I32)
    relb = din("relb", [32, 8])
    n_mp = din("n_mp", [L, D]); n_mpo = din("n_mpo", [L, D])
    n_lp = din("n_lp", [L, D]); n_lpo = din("n_lpo", [L, D])
    w_in = din("w_in", [L, D, DIN])
    dlam = din("dlam", [L, 256]); dsub = din("dsub", [L, 128])
    mqn = din("mqn", [L, 256]); wuq = din("wuq", [L, 256, 768])
    mkn = din("mkn", [L, 128]); wukv = din("wukv", [L, 128, 1024])
    wbr = din("wbr", [L, 1536, D]); wout = din("wout", [L, D, D])
    wup = din("wup", [L, D, DFF]); wdn = din("wdn", [L, DFF, D])
    strips_in = din("strips", [8, 128, SW])
    identb_in = din("identb", [128, 128], BF16)
    identf_in = din("identf", [128, 128])
    sel_in = din("selE", [32, 32 * 128], BF16)
    cstrip_in = din("cstrip", [128, 1024], BF16)
    y_out = nc.dram_tensor("y", [S, D], F32, kind="ExternalOutput").ap()

    hT_d = dscr("hT_d", [D, S], BF16)
    QTd = dscr("QTd", [512, S], BF16); KTd = dscr("KTd", [512, S], BF16)
    QTm = dscr("QTm", [512, S], BF16); KTm = dscr("KTm", [512, S], BF16)
    Vd = dscr("Vd", [S, 4 * 129], BF16); Vm = dscr("Vm", [S, 4 * 129], BF16)
    MBT = dscr("MBT", [128, S], BF16)
    QTc = dscr("QTc", [768, S], BF16); KTc = dscr("KTc", [768, S], BF16)
    Vc = dscr("Vc", [S, 8 * 65], BF16)
    attT = dscr("attT", [1536, S], BF16)
    xa = dscr("xa", [S, D], F32)
    xb = dscr("xb", [S, D], F32)

    es = ExitStack()

    uid = [0]

    def sb(name, shape, dt, st=None):
        uid[0] += 1
        return (st or es).enter_context(nc.sbuf_tensor("sb_%s_%d" % (name, uid[0]), list(shape), dt))

    def ps(name, shape, dt, st=None):
        return (st or es).enter_context(nc.psum_tensor("ps_" + name, list(shape), dt))

    PF = [ps("pf%d" % i, [128, 512], F32) for i in range(6)]
    PW = ps("pw", [128, 1024], F32)
    PB = PF[5][:].bitcast(BF16)

    identb = sb("identb", [128, 128], BF16)
    identf = sb("identf", [128, 128], F32)
    cosT = sb("cosT", [128, NT, 16], F32)
    sinT = sb("sinT", [128, NT, 16], F32)
    lamt = sb("lamt", [128, 8], F32)
    eps_t = sb("eps_t", [128, 1], F32)

    A = sc.add

    with ExitStack() as st:
        posi = sb("posi", [128, NT], I32, st)
        posf = sb("posf", [128, NT], F32, st)
        ang = sb("ang", [128, NT, 16], F32, st)
        A("sp", lambda e: e.dma_start(out=identb[:], in_=identb_in[:, :]), w=["identb"], dma="c0")
        A("sp", lambda e: e.dma_start(out=identf[:], in_=identf_in[:, :]), w=["identf"], dma="c1")
        A("sp", lambda e: e.dma_start(out=posi[:], in_=pos_in[:, :]), w=["posi"], dma="c2")
        A("dve", lambda e: e.tensor_copy(out=posf[:], in_=posi[:]), r=["posi"], w=["posf"])
        A("dve", lambda e: e.memset(eps_t[:], EPS), w=["eps"])
        for i in range(16):
            A("dve", lambda e, i=i: e.tensor_scalar(out=ang[:, :, i], in0=posf[:], scalar1=float(INVF[i]),
                                                    scalar2=None, op0=ALU.mult), r=["posf"], w=[("ang", i)])
        allang = [("ang", i) for i in range(16)]
        ki = sb("ki", [128, NT, 16], I32, st)
        kf = sb("kf", [128, NT, 16], F32, st)
        a2 = sb("a2", [128, NT, 16], F32, st)
        TWO_PI = 2.0 * math.pi
        for dst, shift, key in ((cosT, 0.5 * math.pi, "cosT"), (sinT, 0.0, "sinT")):
            def f1(e, dst=dst, shift=shift):
                e.tensor_scalar(out=a2[:], in0=ang[:], scalar1=float(shift), scalar2=None, op0=ALU.add)
                e.tensor_scalar(out=kf[:], in0=a2[:], scalar1=float(1.0 / TWO_PI), scalar2=None, op0=ALU.mult)
                e.tensor_copy(out=ki[:], in_=kf[:])
                e.tensor_copy(out=kf[:], in_=ki[:])
                e.scalar_tensor_tensor(out=dst[:], in0=kf[:], scalar=float(-TWO_PI), in1=a2[:], op0=ALU.mult, op1=ALU.add)
                e.tensor_scalar(out=kf[:], in0=dst[:], scalar1=float(math.pi), scalar2=float(-TWO_PI),
                                op0=ALU.is_gt, op1=ALU.mult)
                e.tensor_tensor(out=dst[:], in0=dst[:], in1=kf[:], op=ALU.add)
                return e.tensor_scalar(out=dst[:], in0=dst[:], scalar1=float(-math.pi), scalar2=float(math.pi),
                                       op0=ALU.max, op1=ALU.min)
            A("dve", f1, r=allang, w=[key + "0", "rr_tmp"])
            A("act", lambda e, dst=dst: e.activation(out=dst[:], in_=dst[:], func=AF.Sin),
              r=[key + "0"], w=[key])
    sc.barrier()
    if upto == "P0":
        A("pool", lambda e: e.dma_start(out=xa[0:128, 0:16], in_=cosT[:, 0, :]), r=["cosT"], dma="dbg0")
        A("pool", lambda e: e.dma_start(out=xa[0:128, 16:32], in_=sinT[:, 0, :]), r=["sinT"], dma="dbg1")
        A("pool", lambda e: e.dma_start(out=xa[128:256, 0:16], in_=cosT[:, NT - 1, :]), r=["cosT"], dma="dbg2")
        sc.emit(); es.close(); return nc

    def load_cast(dst_ap_fn, src_rows, ncols, stage, nslot, tag, key_w, col_chunk, cast_engs=("pool", "dve")):
        cnt = 0
        for kc, src in enumerate(src_rows):
            for c0 in range(0, ncols, col_chunk):
                cw = min(col_chunk, ncols - c0)
                s = cnt % nslot
                A("sp", lambda e, s=s, src=src, c0=c0, cw=cw: e.dma_start(out=stage[s][:, 0:cw], in_=src[:, c0:c0 + cw]),
                  w=[(tag, s)], dma=(tag, s))
                ce = cast_engs[cnt % len(cast_engs)]
                A(ce, lambda e, s=s, kc=kc, c0=c0, cw=cw: e.tensor_copy(out=dst_ap_fn(kc, c0, cw), in_=stage[s][:, 0:cw]),
                  r=[(tag, s)], w=[key_w])
                cnt += 1

    def rms_rstd(e, out_ap, ss_ap, n):
        e.activation(out=out_ap, in_=ss_ap, func=AF.Ln, scale=1.0 / n, bias=eps_t[0:out_ap.shape[0], 0:1])
        return e.activation(out=out_ap, in_=out_ap, func=AF.Exp, scale=-0.5)

    def bcast_row(dst, src_row_ap, key):
        A("sp", lambda e: e.dma_start(out=dst, in_=src_row_ap.partition_broadcast(128)), w=[key], dma=("bc", key))

    x_cur = x_in
    for l in range(L):
        lam_init = 0.8 - 0.6 * math.exp(-0.3 * l)
        last = (l == L - 1)
        x_fin = y_out if last else xb

        with ExitStack() as st:
            w1 = sb("w1", [128, 8, C_G], BF16, st)
            wuq_b = sb("wuq_b", [128, 2, 768], BF16, st)
            wukv_b = sb("wukv_b", [128, 1024], BF16, st)
            wpre = sb("wpre", [128, D], F32, st)
            mqn_b = sb("mqn_b", [128, 256], F32, st)
            mkn_b = sb("mkn_b", [128, 128], F32, st)
            kmT = sb("kmT", [128, 4, 32], F32, st)
            stage = [sb("stg%d" % i, [128, 872], F32, st) for i in range(2)]
            xt = [sb("xt%d" % i, [128, 4, D], F32, st) for i in range(2)]
            hTt = [sb("hTt%d" % i, [128, 8, 512], BF16, st) for i in range(2)]
            hn = [sb("hn%d" % i, [128, D], BF16, st) for i in range(2)]
            junk = sb("junk", [128, 384], BF16, st)
            ss = sb("ss", [128, NT], F32, st)
            rstd = sb("rstd", [128, NT], F32, st)
            ostg = [sb("ostg%d" % i, [128, 512], BF16, st) for i in range(4)]
            qf = [sb("qf%d" % i, [128, 512], F32, st) for i in range(2)]
            gm = [sb("gm%d" % i, [128, 32], F32, st) for i in range(2)]
            m8 = [sb("m8%d" % i, [128, 8], F32, st) for i in range(2)]
            mbs = [sb("mbs%d" % i, [128, 32], F32, st) for i in range(2)]
            mbTs = [sb("mbTs%d" % i, [32, 512], BF16, st) for i in range(2)]
            vst = [sb("vst%d" % i, [128, 4, 4, 129], BF16, st) for i in range(2)]
            vcst = [sb("vcst%d" % i, [128, 4, 8, 65], BF16, st) for i in range(2)]
            lat = [sb("lat%d" % i, [128, 416], F32, st) for i in range(2)]
            lss = sb("lss", [128, 4 * NT], F32, st)
            latn = [sb("latn%d" % i, [128, 384], BF16, st) for i in range(2)]
            latT = [sb("latT%d" % i, [128, 3, 128], BF16, st) for i in range(2)]
            qcb = [sb("qcb%d" % i, [128, 8, 96], BF16, st) for i in range(2)]
            kcb = [sb("kcb%d" % i, [128, 8, 96], BF16, st) for i in range(2)]
            kr = [sb("kr%d" % i, [128, 32], F32, st) for i in range(2)]
            rt = [sb("rt%d" % i, [128, 8, 16], F32, st) for i in range(4)]
            qcTs = [sb("qcTs%d" % i, [128, 6, 512], BF16, st) for i in range(1)]
            kcTs = [sb("kcTs%d" % i, [128, 6, 512], BF16, st) for i in range(1)]

            load_cast(lambda kc, c0, cw: w1[:, kc, c0:c0 + cw],
                      [w_in[l, kc * 128:(kc + 1) * 128, :] for kc in range(8)], C_G, stage, 2, "wst", "w1", 872)
            load_cast(lambda kc, c0, cw: wuq_b[:, kc, c0:c0 + cw],
                      [wuq[l, kc * 128:(kc + 1) * 128, :] for kc in range(2)], 768, stage, 2, "wst", "wuq", 768)
            load_cast(lambda kc, c0, cw: wukv_b[:, c0:c0 + cw], [wukv[l, :, :]], 1024, stage, 2, "wst", "wukv", 872)
            bcast_row(wpre[:], n_mp[l:l + 1, :], "wpre")
            bcast_row(mqn_b[:], mqn[l:l + 1, :], "mqn")
            bcast_row(mkn_b[:], mkn[l:l + 1, :], "mkn")
            A("dve", lambda e: e.memset(kmT[:], 0.0), w=["kmT"])
            for i in range(2):
                A("pool", lambda e, i=i: e.memset(vst[i][:], 1.0), w=[("vst", i)])
                A("pool", lambda e, i=i: e.memset(vcst[i][:], 1.0), w=[("vcst", i)])

            pfi = [0]

            def nextpf():
                b = pfi[0] % 5
                pfi[0] += 1
                return b

            for t in range(NQ):
                s = t % 2
                A("sp", lambda e, s=s, t=t: e.dma_start(
                    out=xt[s][:], in_=x_cur[t * 512:(t + 1) * 512, :].rearrange("(j p) d -> p j d", p=128)),
                  w=[("xt", s)], dma=("ldx", s))
                for j in range(4):
                    tj = t * 4 + j
                    s2 = tj % 2
                    A("act", lambda e, s=s, j=j, tj=tj: e.activation(out=hn[tj % 2][:], in_=xt[s][:, j, :], func=AF.Square,
                                                                       accum_out=ss[:, tj:tj + 1]),
                      r=[("xt", s)], w=[("ss", tj), ("hn", tj % 2)])
                    A("act", lambda e, tj=tj: rms_rstd(e, rstd[:, tj:tj + 1], ss[:, tj:tj + 1], D),
                      r=[("ss", tj), "eps"], w=[("rstd", tj)])
                    A("dve", lambda e, s=s, j=j, tj=tj, s2=s2: e.scalar_tensor_tensor(
                        out=hn[s2][:], in0=xt[s][:, j, :], scalar=rstd[:, tj:tj + 1], in1=wpre[:],
                        op0=ALU.mult, op1=ALU.mult), r=[("xt", s), ("rstd", tj), "wpre"], w=[("hn", s2)])

                    def tr8(e, s2=s2):
                        for c in range(8):
                            ins = e.transpose(out=PB[:, c * 128:(c + 1) * 128], in_=hn[s2][:, c * 128:(c + 1) * 128],
                                              identity=identb[:])
                        return ins
                    A("pe", tr8, r=[("hn", s2), "identb"], w=["PB"])
                    A("act", lambda e, s=s, j=j: e.copy(out=hTt[s][:, :, j * 128:(j + 1) * 128],
                                                        in_=PB.rearrange("p (c n) -> p c n", c=8)),
                      r=["PB"], w=[("hTt", s)])
                A("pool", lambda e, s=s, t=t: e.dma_start(
                    out=hT_d[:, t * 512:(t + 1) * 512].rearrange("(c p) n -> p c n", p=128), in_=hTt[s][:]),
                  r=[("hTt", s)], w=["hT_d"], dma=("sthT", s))

                def fm_chunk(co, dst, row0, scale, kind, h, oi):
                    b = nextpf()

                    def mm(e, b=b, co=co):
                        for kc in range(8):
                            ins = e.matmul(PF[b][:], lhsT=w1[:, kc, co:co + 128], rhs=hTt[s][:, kc, :],
                                           start=(kc == 0), stop=(kc == 7))
                        return ins
                    A("pe", mm, r=["w1", ("hTt", s)], w=[("PF", b)])
                    so = oi % 4
                    A("act", lambda e, b=b, so=so: e.mul(out=ostg[so][:], in_=PF[b][:], mul=float(scale)),
                      r=[("PF", b)], w=[("ostg", so), ("PFr", b)])
                    A("pool", lambda e, so=so: e.dma_start(out=dst[row0:row0 + 128, t * 512:(t + 1) * 512], in_=ostg[so][:]),
                      r=[("ostg", so)], w=[dst.tensor.name], dma=("sto", so))
                    if kind == "mk":
                        A("dve", lambda e, b=b: e.tensor_reduce(
                            out=kmT[:, h, 2 * t:2 * t + 2], in_=PF[b][:].rearrange("p (a n) -> p a n", a=2),
                            axis=AX.X, op=ALU.add), r=[("PF", b)], w=["kmT", ("PFr", b)])
                    if kind == "mq":
                        sq = h % 2
                        A("dve", lambda e, b=b, sq=sq: e.tensor_copy(out=qf[sq][:], in_=PF[b][:]),
                          r=[("PF", b)], w=[("qf", sq), ("PFr", b)])
                        for j in range(4):
                            own = 2 * t + j // 2
                            sg = (h * 4 + j) % 2
                            bg = nextpf()
                            A("pe", lambda e, bg=bg, sq=sq, j=j: e.matmul(
                                PF[bg][:, 0:32], lhsT=qf[sq][:, j * 128:(j + 1) * 128], rhs=kmT[:, h, :],
                                start=True, stop=True), r=[("qf", sq), "kmT"], w=[("PF", bg)])

                            def sel(e, bg=bg, sg=sg, own=own):
                                e.memset(gm[sg][:], -1e30)
                                if own > 0:
                                    e.tensor_copy(out=gm[sg][:, 0:own], in_=PF[bg][:, 0:own])
                                e.max(out=m8[sg][:], in_=gm[sg][:])
                                e.tensor_scalar(out=m8[sg][:, 2:3], in0=m8[sg][:, 2:3], scalar1=-1e29, scalar2=None,
                                                op0=ALU.max)
                                e.tensor_scalar(out=mbs[sg][:], in0=gm[sg][:], scalar1=m8[sg][:, 2:3], scalar2=None,
                                                op0=ALU.is_ge)
                                e.tensor_scalar(out=mbs[sg][:], in0=mbs[sg][:], scalar1=1.0, scalar2=-NEG,
                                                op0=ALU.subtract, op1=ALU.mult)
                                return e.memset(mbs[sg][:, own:own + 1], 0.0)
                            A("dve", sel, r=[("PF", bg)], w=[("mbs", sg)])
                            bt = nextpf()
                            A("pe", lambda e, bt=bt, sg=sg: e.transpose(out=PF[bt][0:32, 0:128], in_=mbs[sg][:],
                                                                        identity=identf[:]),
                              r=[("mbs", sg), "identf"], w=[("PF", bt)])
                            sm = h % 2
                            A("act", lambda e, bt=bt, sm=sm, j=j: e.copy(out=mbTs[sm][:, j * 128:(j + 1) * 128],
                                                                         in_=PF[bt][0:32, 0:128]),
                              r=[("PF", bt)], w=[("mbTs", sm)])
                        A("pool", lambda e, sm=sm: e.dma_start(out=MBT[h * 32:(h + 1) * 32, t * 512:(t + 1) * 512],
                                                              in_=mbTs[sm][:]),
                          r=[("mbTs", sm)], w=["MBT"], dma=("stmb", sm))

                oi = 0
                for h in range(4):
                    fm_chunk(C_DQ + h * 128, QTd, h * 128, 0.125, "dq", h, oi); oi += 1
                    fm_chunk(C_DK + h * 128, KTd, h * 128, 1.0, "dk", h, oi); oi += 1
                for h in range(4):
                    fm_chunk(C_MK + h * 128, KTm, h * 128, 1.0, "mk", h, oi); oi += 1
                for h in range(4):
                    fm_chunk(C_MQ + h * 128, QTm, h * 128, 1.0 / math.sqrt(128.0), "mq", h, oi); oi += 1

                for gi, (co, dstV) in enumerate(((C_DV, Vd), (C_MV, Vm))):
                    sv = gi
                    for j in range(4):
                        b = nextpf()

                        def mmv(e, b=b, co=co, j=j):
                            for kc in range(8):
                                ins = e.matmul(PF[b][:], lhsT=hTt[s][:, kc, j * 128:(j + 1) * 128],
                                               rhs=w1[:, kc, co:co + 512], start=(kc == 0), stop=(kc == 7))
                            return ins
                        A("pe", mmv, r=["w1", ("hTt", s)], w=[("PF", b)])
                        A("act", lambda e, b=b, sv=sv, j=j: e.copy(out=vst[sv][:, j, :, 0:128],
                                                                   in_=PF[b][:].rearrange("p (h e) -> p h e", h=4)),
                          r=[("PF", b)], w=[("vst", sv)])
                    A("pool", lambda e, sv=sv, dstV=dstV: e.dma_start(
                        out=dstV[t * 512:(t + 1) * 512, :].rearrange("(j p) f -> p j f", p=128),
                        in_=vst[sv][:].rearrange("p j h e -> p j (h e)")),
                      r=[("vst", sv)], w=[dstV.tensor.name], dma=("stv", sv))

                sT = 0
                sV = t % 2
                for j in range(4):
                    tj = t * 4 + j
                    sl = tj % 2
                    b = nextpf()

                    def mml(e, b=b, j=j):
                        for kc in range(8):
                            ins = e.matmul(PF[b][:, 0:416], lhsT=hTt[s][:, kc, j * 128:(j + 1) * 128],
                                           rhs=w1[:, kc, C_CQ:C_CQ + 416], start=(kc == 0), stop=(kc == 7))
                        return ins
                    A("pe", mml, r=["w1", ("hTt", s)], w=[("PF", b)])
                    A("dve", lambda e, b=b, sl=sl: e.tensor_copy(out=lat[sl][:], in_=PF[b][:, 0:416]),
                      r=[("PF", b)], w=[("lat", sl)])

                    def lnorm(e, sl=sl, tj=tj):
                        e.activation(out=junk[:, 0:256], in_=lat[sl][:, 0:256], func=AF.Square,
                                     accum_out=lss[:, 4 * tj:4 * tj + 1])
                        e.activation(out=junk[:, 256:384], in_=lat[sl][:, 256:384], func=AF.Square,
                                     accum_out=lss[:, 4 * tj + 1:4 * tj + 2])
                        rms_rstd(e, lss[:, 4 * tj + 2:4 * tj + 3], lss[:, 4 * tj:4 * tj + 1], 256)
                        return rms_rstd(e, lss[:, 4 * tj + 3:4 * tj + 4], lss[:, 4 * tj + 1:4 * tj + 2], 128)
                    A("act", lnorm, r=[("lat", sl), "eps"], w=[("lss", tj)])

                    def lnorm2(e, sl=sl, tj=tj):
                        e.scalar_tensor_tensor(out=latn[sl][:, 0:256], in0=lat[sl][:, 0:256],
                                               scalar=lss[:, 4 * tj + 2:4 * tj + 3], in1=mqn_b[:],
                                               op0=ALU.mult, op1=ALU.mult)
                        return e.scalar_tensor_tensor(out=latn[sl][:, 256:384], in0=lat[sl][:, 256:384],
                                                      scalar=lss[:, 4 * tj + 3:4 * tj + 4], in1=mkn_b[:],
                                                      op0=ALU.mult, op1=ALU.mult)
                    A("dve", lnorm2, r=[("lat", sl), ("lss", tj), "mqn", "mkn"], w=[("latn", sl)])

                    def tr3(e, sl=sl):
                        for c in range(3):
                            ins = e.transpose(out=PB[:, c * 128:(c + 1) * 128], in_=latn[sl][:, c * 128:(c + 1) * 128],
                                              identity=identb[:])
                        return ins
                    A("pe", tr3, r=[("latn", sl), "identb"], w=["PB"])
                    A("act", lambda e, sl=sl: e.copy(out=latT[sl][:], in_=PB[:, 0:384].rearrange("p (c n) -> p c n", c=3)),
                      r=["PB"], w=[("latT", sl)])

                    def mmq(e, sl=sl):
                        for hf in range(2):
                            for c in range(2):
                                ins = e.matmul(PW[:, hf * 512:hf * 512 + 384], lhsT=latT[sl][:, c, :],
                                               rhs=wuq_b[:, c, hf * 384:(hf + 1) * 384], start=(c == 0), stop=(c == 1))
                        return ins
                    A("pe", mmq, r=[("latT", sl), "wuq"], w=["PW"])
                    cs = cosT[:, tj, :]
                    sn = sinT[:, tj, :]

                    def ropeq(e, sl=sl, cs=cs, sn=sn):
                        for hf in range(2):
                            v = PW[:, hf * 512:hf * 512 + 384].rearrange("p (h e) -> p h e", h=4)
                            x1 = v[:, :, 64:80]
                            x2 = v[:, :, 80:96]
                            cb = cs.unsqueeze(1).broadcast_to([128, 4, 16])
                            sb_ = sn.unsqueeze(1).broadcast_to([128, 4, 16])
                            hs = slice(hf * 4, hf * 4 + 4)
                            e.tensor_tensor(out=rt[0][:, hs, :], in0=x1, in1=cb, op=ALU.mult)
                            e.tensor_tensor(out=rt[1][:, hs, :], in0=x2, in1=sb_, op=ALU.mult)
                            e.tensor_tensor(out=rt[2][:, hs, :], in0=x2, in1=cb, op=ALU.mult)
                            ins = e.tensor_tensor(out=rt[3][:, hs, :], in0=x1, in1=sb_, op=ALU.mult)
                        e.tensor_tensor(out=qcb[sl][:, :, 64:80], in0=rt[0][:], in1=rt[1][:], op=ALU.subtract)
                        return e.tensor_tensor(out=qcb[sl][:, :, 80:96], in0=rt[2][:], in1=rt[3][:], op=ALU.add)
                    A("dve", ropeq, r=["PW", "cosT", "sinT"], w=[("qcb_r", sl), "rt", "PWr"])

                    def qnope(e, sl=sl):
                        for hf in range(2):
                            v = PW[:, hf * 512:hf * 512 + 384].rearrange("p (h e) -> p h e", h=4)
                            ins = e.copy(out=qcb[sl][:, hf * 4:hf * 4 + 4, 0:64], in_=v[:, :, 0:64])
                        return ins
                    A("act", qnope, r=["PW"], w=[("qcb_n", sl), "PWr"])

                    def trq(e, sl=sl):
                        qv = qcb[sl][:].rearrange("p h e -> p (h e)")
                        for c in range(6):
                            ins = e.transpose(out=PB[:, c * 128:(c + 1) * 128], in_=qv[:, c * 128:(c + 1) * 128],
                                              identity=identb[:])
                        return ins
                    A("pe", trq, r=[("qcb_r", sl), ("qcb_n", sl), "identb"], w=["PB"])
                    A("act", lambda e, j=j: e.mul(out=qcTs[sT][:, :, j * 128:(j + 1) * 128],
                                                  in_=PB[:, 0:768].rearrange("p (c n) -> p c n", c=6),
                                                  mul=float(1.0 / math.sqrt(96.0))),
                      r=["PB"], w=[("qcTs", sT)])

                    def mmkv(e, sl=sl):
                        for hf in range(2):
                            ins = e.matmul(PW[:, hf * 512:(hf + 1) * 512], lhsT=latT[sl][:, 2, :],
                                           rhs=wukv_b[:, hf * 512:(hf + 1) * 512], start=True, stop=True)
                        return ins
                    A("pe", mmkv, r=[("latT", sl), "wukv"], w=["PW"])

                    def ropek(e, sl=sl, cs=cs, sn=sn):
                        x1 = lat[sl][:, 384:400]
                        x2 = lat[sl][:, 400:416]
                        e.tensor_tensor(out=rt[0][:, 0, :], in0=x1, in1=cs, op=ALU.mult)
                        e.tensor_tensor(out=rt[1][:, 0, :], in0=x2, in1=sn, op=ALU.mult)
                        e.tensor_tensor(out=rt[2][:, 0, :], in0=x2, in1=cs, op=ALU.mult)
                        e.tensor_tensor(out=rt[3][:, 0, :], in0=x1, in1=sn, op=ALU.mult)
                        e.tensor_tensor(out=kr[sl][:, 0:16], in0=rt[0][:, 0, :], in1=rt[1][:, 0, :], op=ALU.subtract)
                        e.tensor_tensor(out=kr[sl][:, 16:32], in0=rt[2][:, 0, :], in1=rt[3][:, 0, :], op=ALU.add)
                        return e.tensor_copy(out=kcb[sl][:, :, 64:96],
                                             in_=kr[sl][:].unsqueeze(1).broadcast_to([128, 8, 32]))
                    A("dve", ropek, r=[("lat", sl), "cosT", "sinT"], w=[("kcb_r", sl), "rt"])

                    def kvcopy(e, sl=sl, j=j):
                        v = PW[:].rearrange("p (h e) -> p h e", h=8)
                        e.copy(out=kcb[sl][:, :, 0:64], in_=v[:, :, 0:64])
                        return e.copy(out=vcst[sV][:, j, :, 0:64], in_=v[:, :, 64:128])
                    A("act", kvcopy, r=["PW"], w=[("kcb_n", sl), ("vcst", sV)])

                    def trk(e, sl=sl):
                        kv_ = kcb[sl][:].rearrange("p h e -> p (h e)")
                        for c in range(6):
                            ins = e.transpose(out=PB[:, c * 128:(c + 1) * 128], in_=kv_[:, c * 128:(c + 1) * 128],
                                              identity=identb[:])
                        return ins
                    A("pe", trk, r=[("kcb_r", sl), ("kcb_n", sl), "identb"], w=["PB"])
                    A("act", lambda e, j=j: e.copy(out=kcTs[sT][:, :, j * 128:(j + 1) * 128],
                                                   in_=PB[:, 0:768].rearrange("p (c n) -> p c n", c=6)),
                      r=["PB"], w=[("kcTs", sT)])
                A("pool", lambda e, t=t, sT=sT: e.dma_start(
                    out=QTc[:, t * 512:(t + 1) * 512].rearrange("(c p) n -> p c n", p=128), in_=qcTs[sT][:]),
                  r=[("qcTs", sT)], w=["QTc"], dma=("stqc", sT))
                A("pool", lambda e, t=t, sT=sT: e.dma_start(
                    out=KTc[:, t * 512:(t + 1) * 512].rearrange("(c p) n -> p c n", p=128), in_=kcTs[sT][:]),
                  r=[("kcTs", sT)], w=["KTc"], dma=("stkc", sT))
                A("pool", lambda e, t=t, sV=sV: e.dma_start(
                    out=Vc[t * 512:(t + 1) * 512, :].rearrange("(j p) f -> p j f", p=128),
                    in_=vcst[sV][:].rearrange("p j h e -> p j (h e)")),
                  r=[("vcst", sV)], w=["Vc"], dma=("stvc", sV))
        sc.barrier()
        if upto == "P1":
            sc.emit(); es.close(); return nc

        with ExitStack() as st:
            strips = sb("strips", [128, 8, SW], BF16, st)
            cstrip = sb("cstrip", [128, 1024], BF16, st)
            selE = sb("selE", [32, 32 * 128], BF16, st)
            cfar = sb("cfar", [128, 8], F32, st)
            KT = [sb("KT%d" % i, [128, S], BF16, st) for i in range(2)]
            QT = [sb("QT%d" % i, [128, S], BF16, st) for i in range(2)]
            V1 = [sb("V1%d" % i, [128, NT, 129], BF16, st) for i in range(2)]
            MB = sb("MBh", [32, S], BF16, st)
            PT = [sb("PT%d" % i, [128, 512], BF16, st) for i in range(3)]
            Oev = [sb("Oev%d" % i, [128, 4, 129], F32, st) for i in range(2)]
            rcp = [sb("rcp%d" % i, [128, 8], F32, st) for i in range(2)]
            dtl = [sb("dtl%d" % i, [128, 4, 128], F32, st) for i in range(2)]
            dss = sb("dss", [128, 8], F32, st)
            fin = [sb("fin%d" % i, [128, 4, 128], BF16, st) for i in range(2)]
            ast = [sb("ast%d" % i, [128, 512], BF16, st) for i in range(2)]
            lamw = sb("lamw", [128, 256], F32, st)
            wsub = sb("wsub", [128, 128], F32, st)

            with ExitStack() as st2:
                sstage = [sb("sstg%d" % i, [128, SW], F32, st2) for i in range(2)]
                for hb in range(8):
                    s = hb % 2
                    A("sp", lambda e, s=s, hb=hb: e.dma_start(out=sstage[s][:], in_=strips_in[hb, :, :]),
                      w=[("sstg", s)], dma=("sstg", s))
                    A("pool", lambda e, s=s, hb=hb: e.tensor_copy(out=strips[:, hb, :], in_=sstage[s][:]),
                      r=[("sstg", s)], w=["strips"])
                sc.barrier()
            A("sp", lambda e: e.dma_start(out=cstrip[:], in_=cstrip_in[:, :]), w=["cstrip"], dma="c0")
            A("sp", lambda e: e.dma_start(out=selE[:], in_=sel_in[:, :]), w=["selE"], dma="c1")
            bcast_row(cfar[:], relb[31:32, :], "cfar")
            bcast_row(lamw[:], dlam[l:l + 1, :], "lamw")
            bcast_row(wsub[:], dsub[l:l + 1, :], "wsub0")

            def lamf(e):
                e.tensor_tensor(out=lamw[:, 0:64], in0=lamw[:, 0:64], in1=lamw[:, 64:128], op=ALU.mult)
                e.tensor_tensor(out=lamw[:, 128:192], in0=lamw[:, 128:192], in1=lamw[:, 192:256], op=ALU.mult)
                e.tensor_reduce(out=lamt[:, 0:1], in_=lamw[:, 0:64], axis=AX.X, op=ALU.add)
                return e.tensor_reduce(out=lamt[:, 1:2], in_=lamw[:, 128:192], axis=AX.X, op=ALU.add)
            A("dve", lamf, r=["lamw"], w=["lam0"])
            A("act", lambda e: e.activation(out=lamt[:, 0:2], in_=lamt[:, 0:2], func=AF.Exp), r=["lam0"], w=["lam1"])

            def lamg(e):
                e.tensor_tensor(out=lamt[:, 2:3], in0=lamt[:, 1:2], in1=lamt[:, 0:1], op=ALU.subtract)
                e.tensor_scalar(out=lamt[:, 2:3], in0=lamt[:, 2:3], scalar1=float(-lam_init), scalar2=None, op0=ALU.add)
                return e.tensor_scalar(out=wsub[:], in0=wsub[:], scalar1=float(1.0 - lam_init), scalar2=None, op0=ALU.mult)
            A("dve", lamg, r=["lam1", "wsub0"], w=["lam", "wsub"])

            state = {"step": 0, "oset": 0, "head": 0}
            OB = [(PF[3], PF[4]), (PW[:, 0:512], PW[:, 512:1024])]

            def attn_tiles(tiles, hs, dk, dv, bias_kind, hb, mb):
                dv1 = dv + 1
                steps = []
                for ti, (kb, qt, cb) in enumerate(tiles):
                    nk = 4 * (qt + 1)
                    for kc in range(nk):
                        steps.append((ti, kb, qt, kc, kc == nk - 1, cb))
                osets = {}

                def qk(i):
                    ti, kb, qt, kc, lastk, cb = steps[i]
                    g = state["step"] + i
                    b = g % 3
                    dl = qt * 512 - kc * 128
                    near = dl <= 896 if bias_kind == "t5" else dl <= 0

                    def f(e, b=b, kb=kb, qt=qt, kc=kc, dl=dl, near=near):
                        more = near or mb
                        ins = e.matmul(PF[b][:], lhsT=KT[hs][kb:kb + dk, kc * 128:(kc + 1) * 128],
                                       rhs=QT[hs][kb:kb + dk, qt * 512:(qt + 1) * 512], start=True, stop=not more)
                        if near:
                            src = strips[:, hb, dl + 511:dl + 1023] if bias_kind == "t5" else cstrip[:, dl + 511:dl + 1023]
                            ins = e.matmul(PF[b][:], lhsT=identb[:], rhs=src, start=False, stop=not mb)
                        if mb:
                            n = kc // 2
                            ins = e.matmul(PF[b][:], lhsT=selE[:, n * 128:(n + 1) * 128],
                                           rhs=MB[:, qt * 512:(qt + 1) * 512], start=False, stop=True)
                        return ins
                    rr = [("KT", hs), ("QT", hs), "identb", "strips", "cstrip"]
                    if mb:
                        rr += ["selE", "MBh"]
                    A("pe", f, r=rr, w=[("S", b)])
                    return near

                nears = {}
                nears[0] = qk(0)
                for i in range(len(steps)):
                    ti, kb, qt, kc, lastk, cb = steps[i]
                    if i + 1 < len(steps):
                        nears[i + 1] = qk(i + 1)
                    g = state["step"] + i
                    b = g % 3
                    near = nears[i]
                    if kc == 0:
                        osets[ti] = state["oset"] % 2
                        state["oset"] += 1
                    os_ = osets[ti]
                    if bias_kind == "t5" and not near:
                        A("act", lambda e, b=b: e.activation(out=PT[b][:], in_=PF[b][:], func=AF.Exp,
                                                              bias=cfar[:, hb:hb + 1]),
                          r=[("S", b), "cfar"], w=[("PT", b)])
                    else:
                        A("act", lambda e, b=b: e.activation(out=PT[b][:], in_=PF[b][:], func=AF.Exp),
                          r=[("S", b)], w=[("PT", b)])

                    def pv(e, b=b, qt=qt, kc=kc, os_=os_):
                        ins = None
                        for j in range(4):
                            if kc > 4 * qt + j:
                                continue
                            ob = OB[os_][j // 2]
                            c0 = (j % 2) * 129
                            ins = e.matmul(ob[:, c0:c0 + dv1], lhsT=PT[b][:, j * 128:(j + 1) * 128],
                                           rhs=V1[hs][:, kc, 0:dv1], start=(kc == 0 and j % 2 == 0),
                                           stop=(kc == 4 * qt + j), skip_group_check=True)
                        return ins
                    A("pe", pv, r=[("PT", b), ("V1", hs)], w=[("O", os_)])
                    if lastk:
                        A("dve", lambda e, os_=os_: (
                            e.tensor_copy(out=Oev[os_][:, 0:2, 0:dv1],
                                          in_=OB[os_][0][:, 0:258].rearrange("p (a c) -> p a c", a=2)[:, :, 0:dv1]),
                            e.tensor_copy(out=Oev[os_][:, 2:4, 0:dv1],
                                          in_=OB[os_][1][:, 0:258].rearrange("p (a c) -> p a c", a=2)[:, :, 0:dv1]))[1],
                          r=[("O", os_)], w=[("Oev", os_)])
                        cb(os_, qt)
                state["step"] += len(steps)

            def store_fin(fs, nfeat, row0, qt):
                sa = state["head"] % 2
                state["head"] += 1

                def tr(e):
                    for j in range(4):
                        ins = e.transpose(out=PB[0:nfeat, j * 128:(j + 1) * 128], in_=fin[fs][:, j, 0:nfeat],
                                          identity=identb[:])
                    return ins
                A("pe", tr, r=[("fin", fs), "identb"], w=["PB"])
                A("act", lambda e, sa=sa: e.copy(out=ast[sa][0:nfeat, :], in_=PB[0:nfeat, 0:512]),
                  r=["PB"], w=[("ast", sa)])
                A("pool", lambda e, sa=sa: e.dma_start(out=attT[row0:row0 + nfeat, qt * 512:(qt + 1) * 512],
                                                       in_=ast[sa][0:nfeat, :]),
                  r=[("ast", sa)], w=["attT"], dma=("stat", sa))

            def load_head(hs, ktsrc, qtsrc, nrow, vsrc, vc0, dv1, mbsrc=None):
                A("sp", lambda e: e.dma_start(out=KT[hs][0:nrow, :], in_=ktsrc), r=["QTd", "KTd", "QTm", "KTm", "QTc", "KTc"],
                  w=[("KT", hs)], dma=("ldk", hs))
                A("sp", lambda e: e.dma_start(out=QT[hs][0:nrow, :], in_=qtsrc), r=["QTd", "KTd", "QTm", "KTm", "QTc", "KTc"],
                  w=[("QT", hs)], dma=("ldq", hs))
                A("sp", lambda e: e.dma_start(out=V1[hs][:, :, 0:dv1],
                                              in_=vsrc[:, vc0:vc0 + dv1].rearrange("(c p) f -> p c f", p=128)),
                  r=["Vd", "Vm", "Vc"], w=[("V1", hs)], dma=("ldv", hs))
                if mbsrc is not None:
                    A("sp", lambda e: e.dma_start(out=MB[:], in_=mbsrc), r=["MBT"], w=["MBh"], dma="ldmb")

            hcount = 0
            for h in range(4):
                hs = hcount % 2; hcount += 1
                load_head(hs, KTd[h * 128:(h + 1) * 128, :], QTd[h * 128:(h + 1) * 128, :], 128, Vd, h * 129, 129)
                pend = {}

                def cb0(os_, qt):
                    pend[qt] = os_

                def cb1(os_, qt, h=h):
                    o1, o2 = pend[qt], os_
                    fs = qt % 2

                    def comb(e):
                        e.reciprocal(out=rcp[0][:, 0:4], in_=Oev[o1][:, :, 128])
                        e.reciprocal(out=rcp[0][:, 4:8], in_=Oev[o2][:, :, 128])
                        e.tensor_scalar(out=rcp[0][:, 4:8], in0=rcp[0][:, 4:8], scalar1=lamt[:, 2:3], scalar2=None,
                                        op0=ALU.mult)
                        for j in range(4):
                            e.tensor_scalar(out=dtl[0][:, j, :], in0=Oev[o1][:, j, 0:128], scalar1=rcp[0][:, j:j + 1],
                                            scalar2=None, op0=ALU.mult)
                            ins = e.scalar_tensor_tensor(out=dtl[0][:, j, :], in0=Oev[o2][:, j, 0:128],
                                                         scalar=rcp[0][:, 4 + j:5 + j], in1=dtl[0][:, j, :],
                                                         op0=ALU.mult, op1=ALU.add)
                        return ins
                    A("dve", comb, r=[("Oev", o1), ("Oev", o2), "lam"], w=["dtl"])

                    def sq(e):
                        for j in range(4):
                            e.activation(out=dtl[1][:, j, :], in_=dtl[0][:, j, :], func=AF.Square,
                                         accum_out=dss[:, j:j + 1])
                        return rms_rstd(e, dss[:, 4:8], dss[:, 0:4], 128)
                    A("act", sq, r=["dtl", "eps"], w=["dss"])

                    def nrm(e):
                        for j in range(4):
                            ins = e.scalar_tensor_tensor(out=fin[fs][:, j, :], in0=dtl[0][:, j, :],
                                                         scalar=dss[:, 4 + j:5 + j], in1=wsub[:],
                                                         op0=ALU.mult, op1=ALU.mult)
                        return ins
                    A("dve", nrm, r=["dtl", "dss", "wsub"], w=[("fin", fs)])
                    store_fin(fs, 128, h * 128, qt)
                tiles = []
                for qt in range(NQ):
                    tiles.append((0, qt, cb0))
                    tiles.append((64, qt, cb1))
                attn_tiles(tiles, hs, 64, 128, "t5", h, False)

            for h in range(4):
                hs = hcount % 2; hcount += 1
                load_head(hs, KTm[h * 128:(h + 1) * 128, :], QTm[h * 128:(h + 1) * 128, :], 128, Vm, h * 129, 129,
                          MBT[h * 32:(h + 1) * 32, :])

                def cbm(os_, qt, h=h):
                    fs = qt % 2

                    def f(e):
                        e.reciprocal(out=rcp[1][:, 0:4], in_=Oev[os_][:, :, 128])
                        for j in range(4):
                            ins = e.tensor_scalar(out=fin[fs][:, j, :], in0=Oev[os_][:, j, 0:128],
                                                  scalar1=rcp[1][:, j:j + 1], scalar2=None, op0=ALU.mult)
                        return ins
                    A("dve", f, r=[("Oev", os_)], w=[("fin", fs)])
                    store_fin(fs, 128, 512 + h * 128, qt)
                attn_tiles([(0, qt, cbm) for qt in range(NQ)], hs, 128, 128, "t5", 4 + h, True)

            for h in range(8):
                hs = hcount % 2; hcount += 1
                load_head(hs, KTc[h * 96:(h + 1) * 96, :], QTc[h * 96:(h + 1) * 96, :], 96, Vc, h * 65, 65)

                def cbc(os_, qt, h=h):
                    fs = qt % 2

                    def f(e):
                        e.reciprocal(out=rcp[1][:, 0:4], in_=Oev[os_][:, :, 64])
                        for j in range(4):
                            ins = e.tensor_scalar(out=fin[fs][:, j, 0:64], in0=Oev[os_][:, j, 0:64],
                                                  scalar1=rcp[1][:, j:j + 1], scalar2=None, op0=ALU.mult)
                        return ins
                    A("dve", f, r=[("Oev", os_)], w=[("fin", fs)])
                    store_fin(fs, 64, 1024 + h * 64, qt)
                attn_tiles([(0, qt, cbc) for qt in range(NQ)], hs, 96, 64, "causal", 0, False)
        sc.barrier()
        if upto == "P2":
            sc.emit(); es.close(); return nc

        with ExitStack() as st:
            wg = sb("wg", [128, 8, 3072], BF16, st)
            wbr_b = sb("wbr_b", [128, 12, D], BF16, st)
            wout_b = sb("wout_b", [128, 8, D], BF16, st)
            wpost = sb("wpost", [128, D], F32, st)
            with ExitStack() as st2:
                stage = [sb("stg3_%d" % i, [128, 1536], F32, st2) for i in range(2)]
                load_cast(lambda kc, c0, cw: wg[:, kc, c0:c0 + cw],
                          [w_in[l, kc * 128:(kc + 1) * 128, C_G:DIN] for kc in range(8)], 3072, stage, 2, "wst", "wg", 1536)
                load_cast(lambda kc, c0, cw: wbr_b[:, kc, c0:c0 + cw],
                          [wbr[l, kc * 128:(kc + 1) * 128, :] for kc in range(12)], D, stage, 2, "wst", "wbr", D)
                load_cast(lambda kc, c0, cw: wout_b[:, kc, c0:c0 + cw],
                          [wout[l, kc * 128:(kc + 1) * 128, :] for kc in range(8)], D, stage, 2, "wst", "wout", D)
                bcast_row(wpost[:], n_mpo[l:l + 1, :], "wpost")
                sc.barrier()
            hTt = [sb("hTt3_%d" % i, [128, 8, 512], BF16, st) for i in range(2)]
            aTt = [sb("aTt%d" % i, [128, 12, 512], BF16, st) for i in range(2)]
            xt = [sb("xt3_%d" % i, [128, 4, D], F32, st) for i in range(1)]
            mixT = sb("mixT", [128, 8, 512], BF16, st)
            sg = [sb("sg%d" % i, [128, 512], F32, st) for i in range(3)]
            pr = [sb("pr%d" % i, [128, 512], F32, st) for i in range(3)]
            junk = sb("junk3", [128, D], BF16, st)
            ss = sb("ss3", [128, 2 * NT], F32, st)
            tmp = [sb("tmp3_%d" % i, [128, D], F32, st) for i in range(2)]
            gi = 0
            for t in range(NQ):
                s = t % 2
                A("sp", lambda e, s=s, t=t: e.dma_start(
                    out=hTt[s][:], in_=hT_d[:, t * 512:(t + 1) * 512].rearrange("(c p) n -> p c n", p=128)),
                  r=["hT_d"], w=[("hTt", s)], dma=("ldh", s))
                A("sp", lambda e, s=s, t=t: e.dma_start(
                    out=aTt[s][:], in_=attT[:, t * 512:(t + 1) * 512].rearrange("(c p) n -> p c n", p=128)),
                  r=["attT"], w=[("aTt", s)], dma=("lda", s))
                A("sp", lambda e, s=s, t=t: e.dma_start(
                    out=xt[0][:], in_=x_cur[t * 512:(t + 1) * 512, :].rearrange("(j p) d -> p j d", p=128)),
                  r=["xa", "xb"], w=[("xt", 0)], dma=("ldx", 0))
                for oc in range(8):
                    for g in range(3):
                        bg = gi % 6
                        bb = (gi + 1) % 6
                        k3 = (gi // 2) % 3
                        gi += 2

                        def mg(e, bg=bg, g=g, oc=oc):
                            for kc in range(8):
                                ins = e.matmul(PF[bg][:], lhsT=wg[:, kc, g * D + oc * 128:g * D + (oc + 1) * 128],
                                               rhs=hTt[s][:, kc, :], start=(kc == 0), stop=(kc == 7))
                            return ins
                        A("pe", mg, r=["wg", ("hTt", s)], w=[("PF", bg)])

                        def mb_(e, bb=bb, g=g, oc=oc):
                            for c in range(4):
                                ins = e.matmul(PF[bb][:], lhsT=wbr_b[:, g * 4 + c, oc * 128:(oc + 1) * 128],
                                               rhs=aTt[s][:, g * 4 + c, :], start=(c == 0), stop=(c == 3))
                            return ins
                        A("pe", mb_, r=["wbr", ("aTt", s)], w=[("PF", bb)])
                        A("act", lambda e, bg=bg, k3=k3: e.activation(out=sg[k3][:], in_=PF[bg][:], func=AF.Sigmoid),
                          r=[("PF", bg)], w=[("sg", k3)])
                        A("dve", lambda e, bb=bb, k3=k3, g=g: e.tensor_tensor(out=pr[g][:], in0=sg[k3][:], in1=PF[bb][:],
                                                                               op=ALU.mult),
                          r=[("sg", k3), ("PF", bb)], w=[("pr", g)])
                    A("pool", lambda e: e.tensor_tensor(out=pr[0][:], in0=pr[0][:], in1=pr[1][:], op=ALU.add),
                      r=[("pr", 0), ("pr", 1)], w=[("pr", 0)])
                    A("pool", lambda e, oc=oc: e.tensor_tensor(out=mixT[:, oc, :], in0=pr[0][:], in1=pr[2][:], op=ALU.add),
                      r=[("pr", 0), ("pr", 2)], w=[("mixT", oc)])
                for j in range(4):
                    tj = t * 4 + j
                    s2 = tj % 2

                    def mo(e, j=j):
                        for hf in range(2):
                            for kc in range(8):
                                ins = e.matmul(PW[:, hf * 512:(hf + 1) * 512], lhsT=mixT[:, kc, j * 128:(j + 1) * 128],
                                               rhs=wout_b[:, kc, hf * 512:(hf + 1) * 512], start=(kc == 0), stop=(kc == 7))
                        return ins
                    A("pe", mo, r=[("mixT", oc) for oc in range(8)] + ["wout"], w=["PW"])
                    A("act", lambda e, tj=tj: (e.activation(out=junk[:], in_=PW[:], func=AF.Square,
                                                            accum_out=ss[:, 2 * tj:2 * tj + 1]),
                                               rms_rstd(e, ss[:, 2 * tj + 1:2 * tj + 2], ss[:, 2 * tj:2 * tj + 1], D)),
                      r=["PW", "eps"], w=[("ss", tj)])

                    def fz(e, s=s, j=j, tj=tj, s2=s2):
                        return e.scalar_tensor_tensor(out=tmp[s2][:], in0=PW[:], scalar=ss[:, 2 * tj + 1:2 * tj + 2],
                                                      in1=wpost[:], op0=ALU.mult, op1=ALU.mult)
                    A("dve", fz, r=["PW", ("ss", tj), "wpost"], w=[("tmp", s2)])
                    A("pool", lambda e, s=s, j=j, s2=s2: e.tensor_tensor(out=xt[0][:, j, :], in0=xt[0][:, j, :], in1=tmp[s2][:],
                                                                          op=ALU.add),
                      r=[("tmp", s2), ("xt", 0)], w=[("xt", 0)])
                A("pool", lambda e, s=s, t=t: e.dma_start(
                    out=xa[t * 512:(t + 1) * 512, :].rearrange("(j p) d -> p j d", p=128), in_=xt[0][:]),
                  r=[("xt", 0)], w=["xa"], dma=("stx", 0))
        sc.barrier()
        if upto == "P3":
            sc.emit(); es.close(); return nc

        with ExitStack() as st:
            wup_b = sb("wup_b", [128, 8, DFF], BF16, st)
            wdn_b = sb("wdn_b", [128, 32, D], BF16, st)
            wpre = sb("wpre4", [128, D], F32, st)
            wpost = sb("wpost4", [128, D], F32, st)
            with ExitStack() as st2:
                stage = [sb("stg4_%d" % i, [128, 2048], F32, st2) for i in range(2)]
                load_cast(lambda kc, c0, cw: wup_b[:, kc, c0:c0 + cw],
                          [wup[l, kc * 128:(kc + 1) * 128, :] for kc in range(8)], DFF, stage, 2, "wst", "wup", 2048)
                load_cast(lambda kc, c0, cw: wdn_b[:, kc, c0:c0 + cw],
                          [wdn[l, kc * 128:(kc + 1) * 128, :] for kc in range(32)], D, stage, 2, "wst", "wdn", D)
                bcast_row(wpre[:], n_lp[l:l + 1, :], "wpre4")
                bcast_row(wpost[:], n_lpo[l:l + 1, :], "wpost4")
                sc.barrier()
            xt = [sb("xt4_%d" % i, [128, 2, D], F32, st) for i in range(1)]
            hn = [sb("hn4_%d" % i, [128, D], BF16, st) for i in range(2)]
            h2T = sb("h2T", [128, 8, 256], BF16, st)
            uT = sb("uT", [128, 32, 256], BF16, st)
            rl = [sb("rl%d" % i, [128, 256], F32, st) for i in range(2)]
            ss = sb("ss4", [128, 4 * NT], F32, st)
            tmp = [sb("tmp4_%d" % i, [128, D], F32, st) for i in range(1)]
            ui = 0
            for t in range(S // 256):
                s = t % 2
                A("sp", lambda e, s=s, t=t: e.dma_start(
                    out=xt[0][:], in_=xa[t * 256:(t + 1) * 256, :].rearrange("(j p) d -> p j d", p=128)),
                  r=["xa"], w=[("xt", 0)], dma=("ldx", 0))
                for j in range(2):
                    tj = t * 2 + j
                    s2 = tj % 2
                    A("act", lambda e, s=s, j=j, tj=tj: e.activation(out=hn[tj % 2][:], in_=xt[0][:, j, :], func=AF.Square,
                                                                       accum_out=ss[:, 4 * tj:4 * tj + 1]),
                      r=[("xt", 0)], w=[("ssa0", tj), ("hn", tj % 2)])
                    A("act", lambda e, tj=tj: rms_rstd(e, ss[:, 4 * tj + 1:4 * tj + 2], ss[:, 4 * tj:4 * tj + 1], D),
                      r=[("ssa0", tj), "eps"], w=[("ssa", tj)])

                    def nf(e, s=s, j=j, tj=tj, s2=s2):
                        return e.scalar_tensor_tensor(out=hn[s2][:], in0=xt[0][:, j, :], scalar=ss[:, 4 * tj + 1:4 * tj + 2],
                                                      in1=wpre[:], op0=ALU.mult, op1=ALU.mult)
                    A("dve", nf, r=[("xt", 0), ("ssa", tj), "wpre4"], w=[("hn", s2)])

                    def tr8(e, s2=s2):
                        for c in range(8):
                            ins = e.transpose(out=PB[:, c * 128:(c + 1) * 128], in_=hn[s2][:, c * 128:(c + 1) * 128],
                                              identity=identb[:])
                        return ins
                    A("pe", tr8, r=[("hn", s2), "identb"], w=["PB"])
                    A("act", lambda e, j=j: e.copy(out=h2T[:, :, j * 128:(j + 1) * 128],
                                                   in_=PB.rearrange("p (c n) -> p c n", c=8)),
                      r=["PB"], w=["h2T"])
                for fc in range(32):
                    b = ui % 5
                    k2 = ui % 2
                    ui += 1

                    def mu(e, b=b, fc=fc):
                        for kc in range(8):
                            ins = e.matmul(PF[b][:, 0:256], lhsT=wup_b[:, kc, fc * 128:(fc + 1) * 128], rhs=h2T[:, kc, :],
                                           start=(kc == 0), stop=(kc == 7))
                        return ins
                    A("pe", mu, r=["wup", "h2T"], w=[("PF", b)])
                    A("act", lambda e, b=b, k2=k2: e.activation(out=rl[k2][:], in_=PF[b][:, 0:256], func=AF.Relu),
                      r=[("PF", b)], w=[("rl", k2)])
                    A("dve", lambda e, k2=k2, fc=fc: e.tensor_tensor(out=uT[:, fc, :], in0=rl[k2][:], in1=rl[k2][:], op=ALU.mult),
                      r=[("rl", k2)], w=[("uT", fc)])
                for j in range(2):
                    tj = t * 2 + j
                    s2 = tj % 2

                    def md(e, j=j):
                        for hf in range(2):
                            for fc in range(32):
                                ins = e.matmul(PW[:, hf * 512:(hf + 1) * 512], lhsT=uT[:, fc, j * 128:(j + 1) * 128],
                                               rhs=wdn_b[:, fc, hf * 512:(hf + 1) * 512], start=(fc == 0), stop=(fc == 31))
                        return ins
                    A("pe", md, r=[("uT", fc) for fc in range(32)] + ["wdn"], w=["PW"])
                    A("act", lambda e, tj=tj: e.activation(out=tmp[0][:], in_=PW[:], func=AF.Square,
                                                           accum_out=ss[:, 4 * tj + 2:4 * tj + 3]),
                      r=["PW"], w=[("ssb0", tj), ("tmp", 0)])
                    A("act", lambda e, tj=tj: rms_rstd(e, ss[:, 4 * tj + 3:4 * tj + 4], ss[:, 4 * tj + 2:4 * tj + 3], D),
                      r=[("ssb0", tj), "eps"], w=[("ssb", tj)])

                    def fz(e, tj=tj, s2=s2):
                        return e.scalar_tensor_tensor(out=tmp[0][:], in0=PW[:], scalar=ss[:, 4 * tj + 3:4 * tj + 4],
                                                      in1=wpost[:], op0=ALU.mult, op1=ALU.mult)
                    A("dve", fz, r=["PW", ("ssb", tj), "wpost4"], w=[("tmp", 0)])
                    A("pool", lambda e, s=s, j=j, s2=s2: e.tensor_tensor(out=xt[0][:, j, :], in0=xt[0][:, j, :], in1=tmp[0][:],
                                                                          op=ALU.add),
                      r=[("tmp", 0), ("xt", 0)], w=[("xt", 0)])
                A("pool", lambda e, s=s, t=t: e.dma_start(
                    out=x_fin[t * 256:(t + 1) * 256, :].rearrange("(j p) d -> p j d", p=128), in_=xt[0][:]),
                  r=[("xt", 0)], w=["xb"], dma=("stx", 0))
        sc.barrier()
        x_cur = xb

    sc.emit()
    es.close()
    return nc


def _t5_bucket_np(rel):
    n = np.maximum(rel, 0)
    nf = np.maximum(n, 1).astype(np.float32)
    large = 16 + (np.log(nf / np.float32(16)) / np.float32(math.log(64)) * np.float32(16)).astype(np.int32)
    large = np.minimum(large, 31)
    return np.where(n < 16, n, large)


def host_consts(rel_bias):
    kk = np.arange(128)[:, None]
    c = np.arange(SW)[None, :]
    rel = c - kk - 511
    bidx = _t5_bucket_np(rel)
    rb = np.asarray(rel_bias, np.float32)
    strips = np.empty((8, 128, SW), np.float32)
    for h in range(8):
        g = rb[bidx, h]
        strips[h] = np.where(rel >= 0, g, np.float32(NEG))
    c2 = np.arange(1024)[None, :]
    cstrip = np.where(c2 - kk - 511 >= 0, 0.0, NEG).astype(ml_dtypes.bfloat16)
    identb = np.eye(128, dtype=np.float32).astype(ml_dtypes.bfloat16)
    identf = np.eye(128, dtype=np.float32)
    sel = np.zeros((32, 32 * 128), np.float32)
    for n in range(32):
        sel[n, n * 128:(n + 1) * 128] = 1.0
    return dict(strips=strips, cstrip=cstrip, identb=identb, identf=identf, selE=sel.astype(ml_dtypes.bfloat16))


def make_in_maps(x, positions, rel_bias, norm_mix_pre, norm_mix_post, norm_mlp_pre, norm_mlp_post,
                 w_in, diff_lambda, diff_subln, mla_q_norm, mla_w_uq, mla_kv_norm, mla_w_ukv,
                 w_branch, w_out, w_up, w_down, n_cores=8):
    B, S, _ = x.shape
    L = w_in.shape[0]
    f = lambda a: np.ascontiguousarray(np.asarray(a, np.float32))
    shared = dict(
        relb=f(rel_bias), n_mp=f(norm_mix_pre), n_mpo=f(norm_mix_post), n_lp=f(norm_mlp_pre), n_lpo=f(norm_mlp_post),
        w_in=f(w_in), dlam=f(diff_lambda).reshape(L, 256), dsub=f(diff_subln), mqn=f(mla_q_norm), wuq=f(mla_w_uq),
        mkn=f(mla_kv_norm), wukv=f(mla_w_ukv), wbr=f(w_branch).reshape(L, 1536, D), wout=f(w_out), wup=f(w_up),
        wdn=f(w_down))
    shared.update(host_consts(rel_bias))
    maps = []
    for c in range(n_cores):
        b = c % B
        m = dict(shared)
        m["x"] = f(x[b])
        m["pos"] = np.ascontiguousarray(np.asarray(positions[b], np.int32).reshape(S // 128, 128).T)
        maps.append(m)
    return maps


_NC_CACHE = {}


def kernel(**inputs):
    x = np.asarray(inputs["x"])
    B, S, _ = x.shape
    L = np.asarray(inputs["w_in"]).shape[0]
    key = (S, L)
    if key not in _NC_CACHE:
        _NC_CACHE[key] = build(S, L)
    nc = _NC_CACHE[key]
    maps = make_in_maps(**inputs, n_cores=B)
    res = run_bass_kernel_spmd(nc, maps, core_ids=list(range(B)))
    out = np.stack([np.asarray(res.results[b]["y"], np.float32) for b in range(B)], axis=0)
    return out
```

```python
import math
from contextlib import ExitStack

import numpy as np
import ml_dtypes

import concourse.bass as bass
import concourse.mybir as mybir
from concourse.bass_utils import run_bass_kernel_spmd

F32 = mybir.dt.float32
BF16 = mybir.dt.bfloat16
I32 = mybir.dt.int32
AF = mybir.ActivationFunctionType
ALU = mybir.AluOpType
AX = mybir.AxisListType

D = 1024
DIN = 6560
DFF = 4096
EPS = 1e-6
NEG = -30000.0
SW = 1920
C_DQ, C_DK, C_DV, C_MQ, C_MK, C_MV, C_CQ, C_CKV, C_KPE, C_G = (
    0, 512, 1024, 1536, 2048, 2560, 3072, 3328, 3456, 3488)
INVF = [1.0, 0.5623413324356079, 0.3162277638912201, 0.17782793939113617,
        0.10000000149011612, 0.05623413249850273, 0.03162277489900589,
        0.017782794311642647, 0.009999999776482582, 0.005623413249850273,
        0.003162277629598975, 0.0017782794311642647, 0.0010000000474974513,
        0.000562341301701963, 0.0003162277571391314, 0.00017782794020604342]


class _Rec:
    def __init__(self):
        self.calls = []

    def __getattr__(self, name):
        def f(*a, **k):
            self.calls.append((name, a, k))
            return None
        return f


class Sched:
    ENG = ("sp", "act", "pe", "dve", "pool")

    def __init__(self, nc):
        self.nc = nc
        self.ops = []

    def add(self, eng, fn, r=(), w=(), dma=None, par=False):
        rec = _Rec()
        fn(rec)
        assert rec.calls, "op emitted nothing"
        if dma is not None:
            assert len(rec.calls) == 1
        if eng != "pe" and len(rec.calls) > 1 and not par:
            for c in rec.calls:
                self.ops.append([eng, [c], tuple(r), tuple(w), dma])
            return
        self.ops.append([eng, rec.calls, tuple(r), tuple(w), dma])

    def barrier(self):
        self.ops.append(["barrier"])

    def emit(self):
        import os
        if os.environ.get("KTRUNC"):
            self.ops = self.ops[:int(os.environ["KTRUNC"])]
        nc, ops = self.nc, self.ops
        n = len(ops)
        lastw, rd_eng, rd_dma = {}, {}, {}
        deps = [None] * n
        needs = [False] * n
        for i, op in enumerate(ops):
            if op[0] == "barrier":
                continue
            eng, fn, r, w, dma = op
            d = set()
            for k in r:
                if k in lastw:
                    d.add(lastw[k])
            for k in w:
                if k in lastw:
                    d.add(lastw[k])
                d.update(rd_eng.get(k, {}).values())
                d.update(rd_dma.get(k, ()))
            d.discard(i)
            for k in r:
                if dma is None:
                    rd_eng.setdefault(k, {})[eng] = i
                else:
                    rd_dma.setdefault(k, []).append(i)
            for k in w:
                lastw[k] = i
                rd_eng[k] = {}
                rd_dma[k] = []
            dd = []
            for j in sorted(d):
                oj = ops[j]
                if oj[4] is None and dma is None and oj[0] == eng == "pe":
                    continue
                dd.append(j)
                if oj[4] is None:
                    needs[j] = True
            deps[i] = dd
        last_on = {}
        for i, op in enumerate(ops):
            if op[0] == "barrier":
                for j in last_on.values():
                    needs[j] = True
            elif op[4] is None:
                last_on[op[0]] = i
        for j in last_on.values():
            needs[j] = True
        RING = {"sp": 6, "pool": 5}
        cnt = {e: 0 for e in self.ENG}
        dcnt = {}
        ndma = {}
        ring_last = {}
        val = [None] * n
        for i, op in enumerate(ops):
            if op[0] == "barrier":
                op.append((dict(cnt), dict(dcnt)))
                continue
            if op[4] is not None:
                q = op[0]
                k = (q, ndma.get(q, 0) % RING[q])
                ndma[q] = ndma.get(q, 0) + 1
                if k in ring_last:
                    deps[i] = list(deps[i]) + [ring_last[k]]
                ring_last[k] = i
                dcnt[k] = dcnt.get(k, 0) + 16
                val[i] = (("dma", k), dcnt[k])
            elif needs[i]:
                cnt[op[0]] += 1
                val[i] = (("eng", op[0]), cnt[op[0]])
        final = (dict(cnt), dict(dcnt))
        keys = [("eng", e) for e in self.ENG] + [("dma", k) for k in dcnt]
        assert len(keys) <= 16, len(keys)
        with ExitStack() as st:
            sems = {}
            for idx, k in enumerate(keys):
                sems[k] = st.enter_context(nc.semaphore("s%d" % idx))
            with nc.Block() as block:
                def mk(E):
                    def body(e):
                        waited = {}

                        def wait(sk, v):
                            if v > 0 and waited.get(sk, 0) < v:
                                e.wait_ge(sems[sk], v)
                                waited[sk] = v

                        def wait_all(state):
                            cn, dc = state
                            for en, v in cn.items():
                                wait(("eng", en), v)
                            for k, v in dc.items():
                                wait(("dma", k), v)

                        for i, op in enumerate(ops):
                            if op[0] == "barrier":
                                wait_all(op[-1])
                                continue
                            if op[0] != E:
                                continue
                            for j in deps[i]:
                                wait(*val[j])
                            ins = None
                            for (nm, a, k) in op[1]:
                                ins = getattr(e, nm)(*a, **k)
                            if val[i] is not None:
                                ins.then_inc(sems[val[i][0]], 16 if val[i][0][0] == "dma" else 1)
                        wait_all(final)
                    return body
                block.sync(mk("sp"))
                block.scalar(mk("act"))
                block.tensor(mk("pe"))
                block.vector(mk("dve"))
                block.gpsimd(mk("pool"))


def build(S, L, dbg=False, upto=None):
    NT = S // 128
    NQ = S // 512
    NB = S // 256
    nc = bass.Bass("TRN2", target_bir_lowering=False)
    sc = Sched(nc)

    def din(name, shape, dt=F32):
        return nc.dram_tensor(name, list(shape), dt, kind="ExternalInput").ap()

    def dscr(name, shape, dt):
        if dbg:
            return nc.dram_tensor(name, list(shape), dt, kind="ExternalOutput").ap()
        return nc.dram_tensor(name, list(shape), dt).ap()

    x_in = din("x", [S, D])
    pos_in = din("pos", [128, NT], I32)
    relb = din("relb", [32, 8])
    n_mp = din("n_mp", [L, D]); n_mpo = din("n_mpo", [L, D])
    n_lp = din("n_lp", [L, D]); n_lpo = din("n_lpo", [L, D])
    w_in = din("w_in", [L, D, DIN])
    dlam = din("dlam", [L, 256]); dsub = din("dsub", [L, 128])
    mqn = din("mqn", [L, 256]); wuq = din("wuq", [L, 256, 768])
    mkn = din("mkn", [L, 128]); wukv = din("wukv", [L, 128, 1024])
    wbr = din("wbr", [L, 1536, D]); wout = din("wout", [L, D, D])
    wup = din("wup", [L, D, DFF]); wdn = din("wdn", [L, DFF, D])
    strips_in = din("strips", [8, 128, SW])
    identb_in = din("identb", [128, 128], BF16)
    identf_in = din("identf", [128, 128])
    sel_in = din("selE", [32, 32 * 128], BF16)
    cstrip_in = din("cstrip", [128, 1024], BF16)
    y_out = nc.dram_tensor("y", [S, D], F32, kind="ExternalOutput").ap()

    hT_d = dscr("hT_d", [D, S], BF16)
    QTd = dscr("QTd", [512, S], BF16); KTd = dscr("KTd", [512, S], BF16)
    QTm = dscr("QTm", [512, S], BF16); KTm = dscr("KTm", [512, S], BF16)
    Vd = dscr("Vd", [S, 4 * 129], BF16); Vm = dscr("Vm", [S, 4 * 129], BF16)
    MBT = dscr("MBT", [128, S], BF16)
    QTc = dscr("QTc", [768, S], BF16); KTc = dscr("KTc", [768, S], BF16)
    Vc = dscr("Vc", [S, 8 * 65], BF16)
    attT = dscr("attT", [1536, S], BF16)
    xa = dscr("xa", [S, D], F32)
    xb = dscr("xb", [S, D], F32)

    es = ExitStack()

    uid = [0]

    def sb(name, shape, dt, st=None):
        uid[0] += 1
        return (st or es).enter_context(nc.sbuf_tensor("sb_%s_%d" % (name, uid[0]), list(shape), dt))

    def ps(name, shape, dt, st=None):
        return (st or es).enter_context(nc.psum_tensor("ps_" + name, list(shape), dt))

    PF = [ps("pf%d" % i, [128, 512], F32) for i in range(6)]
    PW = ps("pw", [128, 1024], F32)
    PB = PF[5][:].bitcast(BF16)

    identb = sb("identb", [128, 128], BF16)
    identf = sb("identf", [128, 128], F32)
    cosT = sb("cosT", [128, NT, 16], F32)
    sinT = sb("sinT", [128, NT, 16], F32)
    lamt = sb("lamt", [128, 8], F32)
    eps_t = sb("eps_t", [128, 1], F32)

    A = sc.add

    with ExitStack() as st:
        posi = sb("posi", [128, NT], I32, st)
        posf = sb("posf", [128, NT], F32, st)
        ang = sb("ang", [128, NT, 16], F32, st)
        A("sp", lambda e: e.dma_start(out=identb[:], in_=identb_in[:, :]), w=["identb"], dma="c0")
        A("sp", lambda e: e.dma_start(out=identf[:], in_=identf_in[:, :]), w=["identf"], dma="c1")
        A("sp", lambda e: e.dma_start(out=posi[:], in_=pos_in[:, :]), w=["posi"], dma="c2")
        A("dve", lambda e: e.tensor_copy(out=posf[:], in_=posi[:]), r=["posi"], w=["posf"])
        A("dve", lambda e: e.memset(eps_t[:], EPS), w=["eps"])
        for i in range(16):
            A("dve", lambda e, i=i: e.tensor_scalar(out=ang[:, :, i], in0=posf[:], scalar1=float(INVF[i]),
                                                    scalar2=None, op0=ALU.mult), r=["posf"], w=[("ang", i)])
        allang = [("ang", i) for i in range(16)]
        ki = sb("ki", [128, NT, 16], I32, st)
        kf = sb("kf", [128, NT, 16], F32, st)
        a2 = sb("a2", [128, NT, 16], F32, st)
        TWO_PI = 2.0 * math.pi
        for dst, shift, key in ((cosT, 0.5 * math.pi, "cosT"), (sinT, 0.0, "sinT")):
            def f1(e, dst=dst, shift=shift):
                e.tensor_scalar(out=a2[:], in0=ang[:], scalar1=float(shift), scalar2=None, op0=ALU.add)
                e.tensor_scalar(out=kf[:], in0=a2[:], scalar1=float(1.0 / TWO_PI), scalar2=None, op0=ALU.mult)
                e.tensor_copy(out=ki[:], in_=kf[:])
                e.tensor_copy(out=kf[:], in_=ki[:])
                e.scalar_tensor_tensor(out=dst[:], in0=kf[:], scalar=float(-TWO_PI), in1=a2[:], op0=ALU.mult, op1=ALU.add)
                e.tensor_scalar(out=kf[:], in0=dst[:], scalar1=float(math.pi), scalar2=float(-TWO_PI),
                                op0=ALU.is_gt, op1=ALU.mult)
                e.tensor_tensor(out=dst[:], in0=dst[:], in1=kf[:], op=ALU.add)
                return e.tensor_scalar(out=dst[:], in0=dst[:], scalar1=float(-math.pi), scalar2=float(math.pi),
                                       op0=ALU.max, op1=ALU.min)
            A("dve", f1, r=allang, w=[key + "0", "rr_tmp"])
            A("act", lambda e, dst=dst: e.activation(out=dst[:], in_=dst[:], func=AF.Sin),
              r=[key + "0"], w=[key])
    sc.barrier()
    if upto == "P0":
        A("pool", lambda e: e.dma_start(out=xa[0:128, 0:16], in_=cosT[:, 0, :]), r=["cosT"], dma="dbg0")
        A("pool", lambda e: e.dma_start(out=xa[0:128, 16:32], in_=sinT[:, 0, :]), r=["sinT"], dma="dbg1")
        A("pool", lambda e: e.dma_start(out=xa[128:256, 0:16], in_=cosT[:, NT - 1, :]), r=["cosT"], dma="dbg2")
        sc.emit(); es.close(); return nc

    def load_cast(dst_ap_fn, src_rows, ncols, stage, nslot, tag, key_w, col_chunk, cast_engs=("pool", "dve")):
        cnt = 0
        for kc, src in enumerate(src_rows):
            for c0 in range(0, ncols, col_chunk):
                cw = min(col_chunk, ncols - c0)
                s = cnt % nslot
                A("sp", lambda e, s=s, src=src, c0=c0, cw=cw: e.dma_start(out=stage[s][:, 0:cw], in_=src[:, c0:c0 + cw]),
                  w=[(tag, s)], dma=(tag, s))
                ce = cast_engs[cnt % len(cast_engs)]
                A(ce, lambda e, s=s, kc=kc, c0=c0, cw=cw: e.tensor_copy(out=dst_ap_fn(kc, c0, cw), in_=stage[s][:, 0:cw]),
                  r=[(tag, s)], w=[key_w])
                cnt += 1

    def rms_rstd(e, out_ap, ss_ap, n):
        e.activation(out=out_ap, in_=ss_ap, func=AF.Ln, scale=1.0 / n, bias=eps_t[0:out_ap.shape[0], 0:1])
        return e.activation(out=out_ap, in_=out_ap, func=AF.Exp, scale=-0.5)

    def bcast_row(dst, src_row_ap, key):
        A("sp", lambda e: e.dma_start(out=dst, in_=src_row_ap.partition_broadcast(128)), w=[key], dma=("bc", key))

    x_cur = x_in
    for l in range(L):
        lam_init = 0.8 - 0.6 * math.exp(-0.3 * l)
        last = (l == L - 1)
        x_fin = y_out if last else xb

        with ExitStack() as st:
            w1 = sb("w1", [128, 8, C_G], BF16, st)
            wuq_b = sb("wuq_b", [128, 2, 768], BF16, st)
            wukv_b = sb("wukv_b", [128, 1024], BF16, st)
            wpre = sb("wpre", [128, D], F32, st)
            mqn_b = sb("mqn_b", [128, 256], F32, st)
            mkn_b = sb("mkn_b", [128, 128], F32, st)
            kmT = sb("kmT", [128, 4, 32], F32, st)
            vmk = sb("vmk", [128, NB, 32], F32, st)
            stage = [sb("stg%d" % i, [128, 872], F32, st) for i in range(2)]
            xt = [sb("xt%d" % i, [128, 4, D], F32, st) for i in range(2)]
            hTt = [sb("hTt%d" % i, [128, 8, 512], BF16, st) for i in range(2)]
            hn = [sb("hn%d" % i, [128, D], BF16, st) for i in range(2)]
            junk = sb("junk", [128, 384], BF16, st)
            ss = sb("ss", [128, NT], F32, st)
            rstd = sb("rstd", [128, NT], F32, st)
            ostg = [sb("ostg%d" % i, [128, 512], BF16, st) for i in range(4)]
            qf = [sb("qf%d" % i, [128, 512], F32, st) for i in range(2)]
            gm = [sb("gm%d" % i, [128, 32], F32, st) for i in range(2)]
            m8 = [sb("m8%d" % i, [128, 8], F32, st) for i in range(2)]
            mbs = [sb("mbs%d" % i, [128, 32], F32, st) for i in range(2)]
            mbTs = [sb("mbTs%d" % i, [32, 512], BF16, st) for i in range(2)]
            vst = [sb("vst%d" % i, [128, 4, 4, 129], BF16, st) for i in range(2)]
            vcst = [sb("vcst%d" % i, [128, 4, 8, 65], BF16, st) for i in range(2)]
            lat = [sb("lat%d" % i, [128, 416], F32, st) for i in range(2)]
            lss = sb("lss", [128, 4 * NT], F32, st)
            latn = [sb("latn%d" % i, [128, 384], BF16, st) for i in range(2)]
            latT = [sb("latT%d" % i, [128, 3, 128], BF16, st) for i in range(2)]
            qcb = [sb("qcb%d" % i, [128, 8, 96], BF16, st) for i in range(2)]
            kcb = [sb("kcb%d" % i, [128, 8, 96], BF16, st) for i in range(2)]
            kr = [sb("kr%d" % i, [128, 32], F32, st) for i in range(2)]
            rt = [sb("rt%d" % i, [128, 8, 16], F32, st) for i in range(4)]
            qcTs = [sb("qcTs%d" % i, [128, 6, 512], BF16, st) for i in range(1)]
            kcTs = [sb("kcTs%d" % i, [128, 6, 512], BF16, st) for i in range(1)]

            load_cast(lambda kc, c0, cw: w1[:, kc, c0:c0 + cw],
                      [w_in[l, kc * 128:(kc + 1) * 128, :] for kc in range(8)], C_G, stage, 2, "wst", "w1", 872)
            load_cast(lambda kc, c0, cw: wuq_b[:, kc, c0:c0 + cw],
                      [wuq[l, kc * 128:(kc + 1) * 128, :] for kc in range(2)], 768, stage, 2, "wst", "wuq", 768)
            load_cast(lambda kc, c0, cw: wukv_b[:, c0:c0 + cw], [wukv[l, :, :]], 1024, stage, 2, "wst", "wukv", 872)
            bcast_row(wpre[:], n_mp[l:l + 1, :], "wpre")
            bcast_row(mqn_b[:], mqn[l:l + 1, :], "mqn")
            bcast_row(mkn_b[:], mkn[l:l + 1, :], "mkn")
            A("dve", lambda e: e.memset(kmT[:], 0.0), w=["kmT"])
            A("pool", lambda e: e.memset(vmk[:], -1e30), w=["vmk0"])

            def vmf(e):
                for own in range(NB):
                    if own > 0:
                        e.memset(vmk[:, own, 0:own], 0.0)
                    e.memset(vmk[:, own, own:own + 1], 1e30)
            A("pool", vmf, r=["vmk0"], w=["vmk"], par=True)
            for i in range(2):
                A("pool", lambda e, i=i: e.memset(vst[i][:], 1.0), w=[("vst", i)])
                A("pool", lambda e, i=i: e.memset(vcst[i][:], 1.0), w=[("vcst", i)])

            pfi = [0]

            def nextpf():
                b = pfi[0] % 5
                pfi[0] += 1
                return b

            for t in range(NQ):
                s = t % 2
                A("sp", lambda e, s=s, t=t: e.dma_start(
                    out=xt[s][:], in_=x_cur[t * 512:(t + 1) * 512, :].rearrange("(j p) d -> p j d", p=128)),
                  w=[("xt", s)], dma=("ldx", s))
                for j in range(4):
                    tj = t * 4 + j
                    s2 = tj % 2
                    A("act", lambda e, s=s, j=j, tj=tj: e.activation(out=hn[tj % 2][:], in_=xt[s][:, j, :], func=AF.Square,
                                                                       accum_out=ss[:, tj:tj + 1]),
                      r=[("xt", s)], w=[("ss", tj), ("hn", tj % 2)])
                    A("act", lambda e, tj=tj: rms_rstd(e, rstd[:, tj:tj + 1], ss[:, tj:tj + 1], D),
                      r=[("ss", tj), "eps"], w=[("rstd", tj)])
                    A("dve", lambda e, s=s, j=j, tj=tj, s2=s2: e.scalar_tensor_tensor(
                        out=hn[s2][:], in0=xt[s][:, j, :], scalar=rstd[:, tj:tj + 1], in1=wpre[:],
                        op0=ALU.mult, op1=ALU.mult), r=[("xt", s), ("rstd", tj), "wpre"], w=[("hn", s2)])

                    def tr8(e, s2=s2):
                        for c in range(8):
                            ins = e.transpose(out=PB[:, c * 128:(c + 1) * 128], in_=hn[s2][:, c * 128:(c + 1) * 128],
                                              identity=identb[:])
                        return ins
                    A("pe", tr8, r=[("hn", s2), "identb"], w=["PB"])
                    A("act", lambda e, s=s, j=j: e.copy(out=hTt[s][:, :, j * 128:(j + 1) * 128],
                                                        in_=PB.rearrange("p (c n) -> p c n", c=8)),
                      r=["PB"], w=[("hTt", s)])
                A("pool", lambda e, s=s, t=t: e.dma_start(
                    out=hT_d[:, t * 512:(t + 1) * 512].rearrange("(c p) n -> p c n", p=128), in_=hTt[s][:]),
                  r=[("hTt", s)], w=["hT_d"], dma=("sthT", s))

                def fm_chunk(co, dst, row0, scale, kind, h, oi):
                    b = nextpf()

                    def mm(e, b=b, co=co):
                        for kc in range(8):
                            ins = e.matmul(PF[b][:], lhsT=w1[:, kc, co:co + 128], rhs=hTt[s][:, kc, :],
                                           start=(kc == 0), stop=(kc == 7))
                        return ins
                    A("pe", mm, r=["w1", ("hTt", s)], w=[("PF", b)])
                    so = oi % 4
                    A("act", lambda e, b=b, so=so: e.mul(out=ostg[so][:], in_=PF[b][:], mul=float(scale)),
                      r=[("PF", b)], w=[("ostg", so), ("PFr", b)])
                    A("pool", lambda e, so=so: e.dma_start(out=dst[row0:row0 + 128, t * 512:(t + 1) * 512], in_=ostg[so][:]),
                      r=[("ostg", so)], w=[dst.tensor.name], dma=("sto", so))
                    if kind == "mk":
                        A("dve", lambda e, b=b: e.tensor_reduce(
                            out=kmT[:, h, 2 * t:2 * t + 2], in_=PF[b][:].rearrange("p (a n) -> p a n", a=2),
                            axis=AX.X, op=ALU.add), r=[("PF", b)], w=["kmT", ("PFr", b)])
                    if kind == "mq":
                        sq = h % 2
                        A("dve", lambda e, b=b, sq=sq: e.tensor_copy(out=qf[sq][:], in_=PF[b][:]),
                          r=[("PF", b)], w=[("qf", sq), ("PFr", b)])
                        for j in range(4):
                            own = 2 * t + j // 2
                            sg = (h * 4 + j) % 2
                            bg = nextpf()
                            A("pe", lambda e, bg=bg, sq=sq, j=j: e.matmul(
                                PF[bg][:, 0:32], lhsT=qf[sq][:, j * 128:(j + 1) * 128], rhs=kmT[:, h, :],
                                start=True, stop=True), r=[("qf", sq), "kmT"], w=[("PF", bg)])

                            def sel(e, bg=bg, sg=sg, own=own):
                                e.tensor_tensor(out=gm[sg][:], in0=PF[bg][:, 0:32], in1=vmk[:, own, :], op=ALU.add)
                                e.max(out=m8[sg][:], in_=gm[sg][:])
                                e.tensor_scalar(out=m8[sg][:, 3:4], in0=m8[sg][:, 3:4], scalar1=-1e29, scalar2=None,
                                                op0=ALU.max)
                                return e.tensor_scalar(out=mbs[sg][:], in0=gm[sg][:], scalar1=m8[sg][:, 3:4], scalar2=NEG,
                                                       op0=ALU.is_lt, op1=ALU.mult)
                            A("dve", sel, r=[("PF", bg), "vmk"], w=[("mbs", sg)])
                            bt = nextpf()
                            A("pe", lambda e, bt=bt, sg=sg: e.transpose(out=PF[bt][0:32, 0:128], in_=mbs[sg][:],
                                                                        identity=identf[:]),
                              r=[("mbs", sg), "identf"], w=[("PF", bt)])
                            sm = h % 2
                            A("act", lambda e, bt=bt, sm=sm, j=j: e.copy(out=mbTs[sm][:, j * 128:(j + 1) * 128],
                                                                         in_=PF[bt][0:32, 0:128]),
                              r=[("PF", bt)], w=[("mbTs", sm)])
                        A("pool", lambda e, sm=sm: e.dma_start(out=MBT[h * 32:(h + 1) * 32, t * 512:(t + 1) * 512],
                                                              in_=mbTs[sm][:]),
                          r=[("mbTs", sm)], w=["MBT"], dma=("stmb", sm))

                oi = 0
                for h in range(4):
                    fm_chunk(C_DQ + h * 128, QTd, h * 128, 0.125, "dq", h, oi); oi += 1
                    fm_chunk(C_DK + h * 128, KTd, h * 128, 1.0, "dk", h, oi); oi += 1
                for h in range(4):
                    fm_chunk(C_MK + h * 128, KTm, h * 128, 1.0, "mk", h, oi); oi += 1
                for h in range(4):
                    fm_chunk(C_MQ + h * 128, QTm, h * 128, 1.0 / math.sqrt(128.0), "mq", h, oi); oi += 1

                for gi, (co, dstV) in enumerate(((C_DV, Vd), (C_MV, Vm))):
                    sv = gi
                    for j in range(4):
                        b = nextpf()

                        def mmv(e, b=b, co=co, j=j):
                            for kc in range(8):
                                ins = e.matmul(PF[b][:], lhsT=hTt[s][:, kc, j * 128:(j + 1) * 128],
                                               rhs=w1[:, kc, co:co + 512], start=(kc == 0), stop=(kc == 7))
                            return ins
                        A("pe", mmv, r=["w1", ("hTt", s)], w=[("PF", b)])
                        A("act", lambda e, b=b, sv=sv, j=j: e.copy(out=vst[sv][:, j, :, 0:128],
                                                                   in_=PF[b][:].rearrange("p (h e) -> p h e", h=4)),
                          r=[("PF", b)], w=[("vst", sv)])
                    A("pool", lambda e, sv=sv, dstV=dstV: e.dma_start(
                        out=dstV[t * 512:(t + 1) * 512, :].rearrange("(j p) f -> p j f", p=128),
                        in_=vst[sv][:].rearrange("p j h e -> p j (h e)")),
                      r=[("vst", sv)], w=[dstV.tensor.name], dma=("stv", sv))

                sT = 0
                sV = t % 2
                for j in range(4):
                    tj = t * 4 + j
                    sl = tj % 2
                    b = nextpf()

                    def mml(e, b=b, j=j):
                        for kc in range(8):
                            ins = e.matmul(PF[b][:, 0:416], lhsT=hTt[s][:, kc, j * 128:(j + 1) * 128],
                                           rhs=w1[:, kc, C_CQ:C_CQ + 416], start=(kc == 0), stop=(kc == 7))
                        return ins
                    A("pe", mml, r=["w1", ("hTt", s)], w=[("PF", b)])
                    A("dve", lambda e, b=b, sl=sl: e.tensor_copy(out=lat[sl][:], in_=PF[b][:, 0:416]),
                      r=[("PF", b)], w=[("lat", sl)])

                    def lnorm(e, sl=sl, tj=tj):
                        e.activation(out=junk[:, 0:256], in_=lat[sl][:, 0:256], func=AF.Square,
                                     accum_out=lss[:, 4 * tj:4 * tj + 1])
                        e.activation(out=junk[:, 256:384], in_=lat[sl][:, 256:384], func=AF.Square,
                                     accum_out=lss[:, 4 * tj + 1:4 * tj + 2])
                    A("act", lnorm, r=[("lat", sl)], w=[("lss0", tj)], par=True)

                    def lnormb(e, tj=tj):
                        e.activation(out=lss[:, 4 * tj + 2:4 * tj + 3], in_=lss[:, 4 * tj:4 * tj + 1], func=AF.Ln,
                                     scale=1.0 / 256, bias=eps_t[:, 0:1])
                        e.activation(out=lss[:, 4 * tj + 3:4 * tj + 4], in_=lss[:, 4 * tj + 1:4 * tj + 2], func=AF.Ln,
                                     scale=1.0 / 128, bias=eps_t[:, 0:1])
                    A("act", lnormb, r=[("lss0", tj), "eps"], w=[("lss1", tj)], par=True)
                    A("act", lambda e, tj=tj: e.activation(out=lss[:, 4 * tj + 2:4 * tj + 4], in_=lss[:, 4 * tj + 2:4 * tj + 4],
                                                           func=AF.Exp, scale=-0.5),
                      r=[("lss1", tj)], w=[("lss", tj)])

                    def lnorm2(e, sl=sl, tj=tj):
                        e.scalar_tensor_tensor(out=latn[sl][:, 0:256], in0=lat[sl][:, 0:256],
                                               scalar=lss[:, 4 * tj + 2:4 * tj + 3], in1=mqn_b[:],
                                               op0=ALU.mult, op1=ALU.mult)
                        return e.scalar_tensor_tensor(out=latn[sl][:, 256:384], in0=lat[sl][:, 256:384],
                                                      scalar=lss[:, 4 * tj + 3:4 * tj + 4], in1=mkn_b[:],
                                                      op0=ALU.mult, op1=ALU.mult)
                    A("dve", lnorm2, r=[("lat", sl), ("lss", tj), "mqn", "mkn"], w=[("latn", sl)], par=True)

                    def tr3(e, sl=sl):
                        for c in range(3):
                            ins = e.transpose(out=PB[:, c * 128:(c + 1) * 128], in_=latn[sl][:, c * 128:(c + 1) * 128],
                                              identity=identb[:])
                        return ins
                    A("pe", tr3, r=[("latn", sl), "identb"], w=["PB"])
                    A("act", lambda e, sl=sl: e.copy(out=latT[sl][:], in_=PB[:, 0:384].rearrange("p (c n) -> p c n", c=3)),
                      r=["PB"], w=[("latT", sl)])

                    def mmq(e, sl=sl):
                        for hf in range(2):
                            for c in range(2):
                                ins = e.matmul(PW[:, hf * 512:hf * 512 + 384], lhsT=latT[sl][:, c, :],
                                               rhs=wuq_b[:, c, hf * 384:(hf + 1) * 384], start=(c == 0), stop=(c == 1))
                        return ins
                    A("pe", mmq, r=[("latT", sl), "wuq"], w=["PW"])
                    cs = cosT[:, tj, :]
                    sn = sinT[:, tj, :]

                    def ropeq(e, sl=sl, cs=cs, sn=sn):
                        for hf in range(2):
                            v = PW[:, hf * 512:hf * 512 + 384].rearrange("p (h e) -> p h e", h=4)
                            x1 = v[:, :, 64:80]
                            x2 = v[:, :, 80:96]
                            cb = cs.unsqueeze(1).broadcast_to([128, 4, 16])
                            sb_ = sn.unsqueeze(1).broadcast_to([128, 4, 16])
                            hs = slice(hf * 4, hf * 4 + 4)
                            e.tensor_tensor(out=rt[0][:, hs, :], in0=x1, in1=cb, op=ALU.mult)
                            e.tensor_tensor(out=rt[1][:, hs, :], in0=x2, in1=sb_, op=ALU.mult)
                            e.tensor_tensor(out=rt[2][:, hs, :], in0=x2, in1=cb, op=ALU.mult)
                            ins = e.tensor_tensor(out=rt[3][:, hs, :], in0=x1, in1=sb_, op=ALU.mult)
                    A("dve", ropeq, r=["PW", "cosT", "sinT"], w=["rt", "PWr"], par=True)

                    def ropeq2(e, sl=sl):
                        e.tensor_tensor(out=qcb[sl][:, :, 64:80], in0=rt[0][:], in1=rt[1][:], op=ALU.subtract)
                        return e.tensor_tensor(out=qcb[sl][:, :, 80:96], in0=rt[2][:], in1=rt[3][:], op=ALU.add)
                    A("dve", ropeq2, r=["rt"], w=[("qcb_r", sl)], par=True)

                    def qnope(e, sl=sl):
                        for hf in range(2):
                            v = PW[:, hf * 512:hf * 512 + 384].rearrange("p (h e) -> p h e", h=4)
                            ins = e.copy(out=qcb[sl][:, hf * 4:hf * 4 + 4, 0:64], in_=v[:, :, 0:64])
                        return ins
                    A("act", qnope, r=["PW"], w=[("qcb_n", sl), "PWr"], par=True)

                    def trq(e, sl=sl):
                        qv = qcb[sl][:].rearrange("p h e -> p (h e)")
                        for c in range(6):
                            ins = e.transpose(out=PB[:, c * 128:(c + 1) * 128], in_=qv[:, c * 128:(c + 1) * 128],
                                              identity=identb[:])
                        return ins
                    A("pe", trq, r=[("qcb_r", sl), ("qcb_n", sl), "identb"], w=["PB"])
                    A("act", lambda e, j=j: e.mul(out=qcTs[sT][:, :, j * 128:(j + 1) * 128],
                                                  in_=PB[:, 0:768].rearrange("p (c n) -> p c n", c=6),
                                                  mul=float(1.0 / math.sqrt(96.0))),
                      r=["PB"], w=[("qcTs", sT)])

                    def mmkv(e, sl=sl):
                        for hf in range(2):
                            ins = e.matmul(PW[:, hf * 512:(hf + 1) * 512], lhsT=latT[sl][:, 2, :],
                                           rhs=wukv_b[:, hf * 512:(hf + 1) * 512], start=True, stop=True)
                        return ins
                    A("pe", mmkv, r=[("latT", sl), "wukv"], w=["PW"])

                    def ropek(e, sl=sl, cs=cs, sn=sn):
                        x1 = lat[sl][:, 384:400]
                        x2 = lat[sl][:, 400:416]
                        e.tensor_tensor(out=rt[0][:, 0, :], in0=x1, in1=cs, op=ALU.mult)
                        e.tensor_tensor(out=rt[1][:, 0, :], in0=x2, in1=sn, op=ALU.mult)
                        e.tensor_tensor(out=rt[2][:, 0, :], in0=x2, in1=cs, op=ALU.mult)
                        e.tensor_tensor(out=rt[3][:, 0, :], in0=x1, in1=sn, op=ALU.mult)
                    A("dve", ropek, r=[("lat", sl), "cosT", "sinT"], w=["rt"], par=True)

                    def ropek2(e, sl=sl):
                        e.tensor_tensor(out=kr[sl][:, 0:16], in0=rt[0][:, 0, :], in1=rt[1][:, 0, :], op=ALU.subtract)
                        e.tensor_tensor(out=kr[sl][:, 16:32], in0=rt[2][:, 0, :], in1=rt[3][:, 0, :], op=ALU.add)
                    A("dve", ropek2, r=["rt"], w=[("kr", sl)], par=True)
                    A("dve", lambda e, sl=sl: e.tensor_copy(out=kcb[sl][:, :, 64:96],
                                                            in_=kr[sl][:].unsqueeze(1).broadcast_to([128, 8, 32])),
                      r=[("kr", sl)], w=[("kcb_r", sl)])

                    def kvcopy(e, sl=sl, j=j):
                        v = PW[:].rearrange("p (h e) -> p h e", h=8)
                        e.copy(out=kcb[sl][:, :, 0:64], in_=v[:, :, 0:64])
                        return e.copy(out=vcst[sV][:, j, :, 0:64], in_=v[:, :, 64:128])
                    A("act", kvcopy, r=["PW"], w=[("kcb_n", sl), ("vcst", sV)], par=True)

                    def trk(e, sl=sl):
                        kv_ = kcb[sl][:].rearrange("p h e -> p (h e)")
                        for c in range(6):
                            ins = e.transpose(out=PB[:, c * 128:(c + 1) * 128], in_=kv_[:, c * 128:(c + 1) * 128],
                                              identity=identb[:])
                        return ins
                    A("pe", trk, r=[("kcb_r", sl), ("kcb_n", sl), "identb"], w=["PB"])
                    A("act", lambda e, j=j: e.copy(out=kcTs[sT][:, :, j * 128:(j + 1) * 128],
                                                   in_=PB[:, 0:768].rearrange("p (c n) -> p c n", c=6)),
                      r=["PB"], w=[("kcTs", sT)])
                A("pool", lambda e, t=t, sT=sT: e.dma_start(
                    out=QTc[:, t * 512:(t + 1) * 512].rearrange("(c p) n -> p c n", p=128), in_=qcTs[sT][:]),
                  r=[("qcTs", sT)], w=["QTc"], dma=("stqc", sT))
                A("pool", lambda e, t=t, sT=sT: e.dma_start(
                    out=KTc[:, t * 512:(t + 1) * 512].rearrange("(c p) n -> p c n", p=128), in_=kcTs[sT][:]),
                  r=[("kcTs", sT)], w=["KTc"], dma=("stkc", sT))
                A("pool", lambda e, t=t, sV=sV: e.dma_start(
                    out=Vc[t * 512:(t + 1) * 512, :].rearrange("(j p) f -> p j f", p=128),
                    in_=vcst[sV][:].rearrange("p j h e -> p j (h e)")),
                  r=[("vcst", sV)], w=["Vc"], dma=("stvc", sV))
        sc.barrier()
        if upto == "P1":
            sc.emit(); es.close(); return nc

        with ExitStack() as st:
            strips = sb("strips", [128, 8, SW], BF16, st)
            cstrip = sb("cstrip", [128, 1024], BF16, st)
            selE = sb("selE", [32, 32 * 128], BF16, st)
            cfar = sb("cfar", [128, 8], F32, st)
            KT = [sb("KT%d" % i, [128, S], BF16, st) for i in range(2)]
            QT = [sb("QT%d" % i, [128, S], BF16, st) for i in range(2)]
            V1 = [sb("V1%d" % i, [128, NT, 129], BF16, st) for i in range(2)]
            MB = sb("MBh", [32, S], BF16, st)
            PT = [sb("PT%d" % i, [128, 512], BF16, st) for i in range(3)]
            Oev = [sb("Oev%d" % i, [128, 4, 129], F32, st) for i in range(2)]
            rcp = [sb("rcp%d" % i, [128, 8], F32, st) for i in range(2)]
            dtl = [sb("dtl%d" % i, [128, 4, 128], F32, st) for i in range(2)]
            dss = sb("dss", [128, 8], F32, st)
            fin = [sb("fin%d" % i, [128, 4, 128], BF16, st) for i in range(2)]
            ast = [sb("ast%d" % i, [128, 512], BF16, st) for i in range(2)]
            lamw = sb("lamw", [128, 256], F32, st)
            wsub = sb("wsub", [128, 128], F32, st)

            with ExitStack() as st2:
                sstage = [sb("sstg%d" % i, [128, SW], F32, st2) for i in range(2)]
                for hb in range(8):
                    s = hb % 2
                    A("sp", lambda e, s=s, hb=hb: e.dma_start(out=sstage[s][:], in_=strips_in[hb, :, :]),
                      w=[("sstg", s)], dma=("sstg", s))
                    A("pool", lambda e, s=s, hb=hb: e.tensor_copy(out=strips[:, hb, :], in_=sstage[s][:]),
                      r=[("sstg", s)], w=["strips"])
                sc.barrier()
            A("sp", lambda e: e.dma_start(out=cstrip[:], in_=cstrip_in[:, :]), w=["cstrip"], dma="c0")
            A("sp", lambda e: e.dma_start(out=selE[:], in_=sel_in[:, :]), w=["selE"], dma="c1")
            bcast_row(cfar[:], relb[31:32, :], "cfar")
            bcast_row(lamw[:], dlam[l:l + 1, :], "lamw")
            bcast_row(wsub[:], dsub[l:l + 1, :], "wsub0")

            def lamf(e):
                e.tensor_tensor(out=lamw[:, 0:64], in0=lamw[:, 0:64], in1=lamw[:, 64:128], op=ALU.mult)
                e.tensor_tensor(out=lamw[:, 128:192], in0=lamw[:, 128:192], in1=lamw[:, 192:256], op=ALU.mult)
                e.tensor_reduce(out=lamt[:, 0:1], in_=lamw[:, 0:64], axis=AX.X, op=ALU.add)
                return e.tensor_reduce(out=lamt[:, 1:2], in_=lamw[:, 128:192], axis=AX.X, op=ALU.add)
            A("dve", lamf, r=["lamw"], w=["lam0"])
            A("act", lambda e: e.activation(out=lamt[:, 0:2], in_=lamt[:, 0:2], func=AF.Exp), r=["lam0"], w=["lam1"])

            def lamg(e):
                e.tensor_tensor(out=lamt[:, 2:3], in0=lamt[:, 1:2], in1=lamt[:, 0:1], op=ALU.subtract)
                e.tensor_scalar(out=lamt[:, 2:3], in0=lamt[:, 2:3], scalar1=float(-lam_init), scalar2=None, op0=ALU.add)
                return e.tensor_scalar(out=wsub[:], in0=wsub[:], scalar1=float(1.0 - lam_init), scalar2=None, op0=ALU.mult)
            A("dve", lamg, r=["lam1", "wsub0"], w=["lam", "wsub"])

            state = {"step": 0, "oset": 0, "head": 0}
            OB = [(PF[3], PF[4]), (PW[:, 0:512], PW[:, 512:1024])]

            def attn_tiles(tiles, hs, dk, dv, bias_kind, hb, mb):
                dv1 = dv + 1
                steps = []
                for ti, (kb, qt, cb) in enumerate(tiles):
                    nk = 4 * (qt + 1)
                    for kc in range(nk):
                        steps.append((ti, kb, qt, kc, kc == nk - 1, cb))
                osets = {}

                def qk(i):
                    ti, kb, qt, kc, lastk, cb = steps[i]
                    g = state["step"] + i
                    b = g % 3
                    dl = qt * 512 - kc * 128
                    near = dl <= 896 if bias_kind == "t5" else dl <= 0

                    def f(e, b=b, kb=kb, qt=qt, kc=kc, dl=dl, near=near):
                        more = near or mb
                        ins = e.matmul(PF[b][:], lhsT=KT[hs][kb:kb + dk, kc * 128:(kc + 1) * 128],
                                       rhs=QT[hs][kb:kb + dk, qt * 512:(qt + 1) * 512], start=True, stop=not more)
                        if near:
                            src = strips[:, hb, dl + 511:dl + 1023] if bias_kind == "t5" else cstrip[:, dl + 511:dl + 1023]
                            ins = e.matmul(PF[b][:], lhsT=identb[:], rhs=src, start=False, stop=not mb)
                        if mb:
                            n = kc // 2
                            ins = e.matmul(PF[b][:], lhsT=selE[:, n * 128:(n + 1) * 128],
                                           rhs=MB[:, qt * 512:(qt + 1) * 512], start=False, stop=True)
                        return ins
                    rr = [("KT", hs), ("QT", hs), "identb", "strips", "cstrip"]
                    if mb:
                        rr += ["selE", "MBh"]
                    A("pe", f, r=rr, w=[("S", b)])
                    return near

                nears = {}
                nears[0] = qk(0)
                for i in range(len(steps)):
                    ti, kb, qt, kc, lastk, cb = steps[i]
                    if i + 1 < len(steps):
                        nears[i + 1] = qk(i + 1)
                    g = state["step"] + i
                    b = g % 3
                    near = nears[i]
                    if kc == 0:
                        osets[ti] = state["oset"] % 2
                        state["oset"] += 1
                    os_ = osets[ti]
                    if bias_kind == "t5" and not near:
                        A("act", lambda e, b=b: e.activation(out=PT[b][:], in_=PF[b][:], func=AF.Exp,
                                                              bias=cfar[:, hb:hb + 1]),
                          r=[("S", b), "cfar"], w=[("PT", b)])
                    else:
                        A("act", lambda e, b=b: e.activation(out=PT[b][:], in_=PF[b][:], func=AF.Exp),
                          r=[("S", b)], w=[("PT", b)])

                    def pv(e, b=b, qt=qt, kc=kc, os_=os_):
                        ins = None
                        for j in range(4):
                            if kc > 4 * qt + j:
                                continue
                            ob = OB[os_][j // 2]
                            c0 = (j % 2) * 129
                            ins = e.matmul(ob[:, c0:c0 + dv1], lhsT=PT[b][:, j * 128:(j + 1) * 128],
                                           rhs=V1[hs][:, kc, 0:dv1], start=(kc == 0 and j % 2 == 0),
                                           stop=(kc == 4 * qt + j), skip_group_check=True)
                        return ins
                    A("pe", pv, r=[("PT", b), ("V1", hs)], w=[("O", os_)])
                    if lastk:
                        A("dve", lambda e, os_=os_: (
                            e.tensor_copy(out=Oev[os_][:, 0:2, 0:dv1],
                                          in_=OB[os_][0][:, 0:258].rearrange("p (a c) -> p a c", a=2)[:, :, 0:dv1]),
                            e.tensor_copy(out=Oev[os_][:, 2:4, 0:dv1],
                                          in_=OB[os_][1][:, 0:258].rearrange("p (a c) -> p a c", a=2)[:, :, 0:dv1]))[1],
                          r=[("O", os_)], w=[("Oev", os_)], par=True)
                        cb(os_, qt)
                state["step"] += len(steps)

            def store_fin(fs, nfeat, row0, qt):
                sa = state["head"] % 2
                state["head"] += 1

                def tr(e):
                    for j in range(4):
                        ins = e.transpose(out=PB[0:nfeat, j * 128:(j + 1) * 128], in_=fin[fs][:, j, 0:nfeat],
                                          identity=identb[:])
                    return ins
                A("pe", tr, r=[("fin", fs), "identb"], w=["PB"])
                A("act", lambda e, sa=sa: e.copy(out=ast[sa][0:nfeat, :], in_=PB[0:nfeat, 0:512]),
                  r=["PB"], w=[("ast", sa)])
                A("pool", lambda e, sa=sa: e.dma_start(out=attT[row0:row0 + nfeat, qt * 512:(qt + 1) * 512],
                                                       in_=ast[sa][0:nfeat, :]),
                  r=[("ast", sa)], w=["attT"], dma=("stat", sa))

            def load_head(hs, ktsrc, qtsrc, nrow, vsrc, vc0, dv1, mbsrc=None):
                A("sp", lambda e: e.dma_start(out=KT[hs][0:nrow, :], in_=ktsrc), r=["QTd", "KTd", "QTm", "KTm", "QTc", "KTc"],
                  w=[("KT", hs)], dma=("ldk", hs))
                A("sp", lambda e: e.dma_start(out=QT[hs][0:nrow, :], in_=qtsrc), r=["QTd", "KTd", "QTm", "KTm", "QTc", "KTc"],
                  w=[("QT", hs)], dma=("ldq", hs))
                A("sp", lambda e: e.dma_start(out=V1[hs][:, :, 0:dv1],
                                              in_=vsrc[:, vc0:vc0 + dv1].rearrange("(c p) f -> p c f", p=128)),
                  r=["Vd", "Vm", "Vc"], w=[("V1", hs)], dma=("ldv", hs))
                if mbsrc is not None:
                    A("sp", lambda e: e.dma_start(out=MB[:], in_=mbsrc), r=["MBT"], w=["MBh"], dma="ldmb")

            hcount = 0
            for h in range(4):
                hs = hcount % 2; hcount += 1
                load_head(hs, KTd[h * 128:(h + 1) * 128, :], QTd[h * 128:(h + 1) * 128, :], 128, Vd, h * 129, 129)
                pend = {}

                def cb0(os_, qt):
                    pend[qt] = os_

                def cb1(os_, qt, h=h):
                    o1, o2 = pend[qt], os_
                    fs = qt % 2

                    def comb0(e):
                        e.reciprocal(out=rcp[0][:, 0:4], in_=Oev[o1][:, :, 128])
                        e.reciprocal(out=rcp[0][:, 4:8], in_=Oev[o2][:, :, 128])
                    A("dve", comb0, r=[("Oev", o1), ("Oev", o2)], w=["rcp0"], par=True)
                    A("dve", lambda e: e.tensor_scalar(out=rcp[0][:, 4:8], in0=rcp[0][:, 4:8], scalar1=lamt[:, 2:3],
                                                       scalar2=None, op0=ALU.mult), r=["rcp0", "lam"], w=["rcp0b"])

                    def comb1(e):
                        for j in range(4):
                            e.tensor_scalar(out=dtl[0][:, j, :], in0=Oev[o1][:, j, 0:128], scalar1=rcp[0][:, j:j + 1],
                                            scalar2=None, op0=ALU.mult)
                    A("dve", comb1, r=[("Oev", o1), "rcp0b"], w=["dtl0"], par=True)

                    def comb2(e):
                        for j in range(4):
                            e.scalar_tensor_tensor(out=dtl[0][:, j, :], in0=Oev[o2][:, j, 0:128],
                                                   scalar=rcp[0][:, 4 + j:5 + j], in1=dtl[0][:, j, :],
                                                   op0=ALU.mult, op1=ALU.add)
                    A("dve", comb2, r=[("Oev", o2), "rcp0b", "dtl0"], w=["dtl"], par=True)

                    def sq(e):
                        for j in range(4):
                            e.activation(out=dtl[1][:, j, :], in_=dtl[0][:, j, :], func=AF.Square,
                                         accum_out=dss[:, j:j + 1])
                    A("act", sq, r=["dtl"], w=["dss0"], par=True)
                    A("act", lambda e: rms_rstd(e, dss[:, 4:8], dss[:, 0:4], 128), r=["dss0", "eps"], w=["dss"])

                    def nrm(e):
                        for j in range(4):
                            ins = e.scalar_tensor_tensor(out=fin[fs][:, j, :], in0=dtl[0][:, j, :],
                                                         scalar=dss[:, 4 + j:5 + j], in1=wsub[:],
                                                         op0=ALU.mult, op1=ALU.mult)
                        return ins
                    A("dve", nrm, r=["dtl", "dss", "wsub"], w=[("fin", fs)], par=True)
                    store_fin(fs, 128, h * 128, qt)
                tiles = []
                for qt in range(NQ):
                    tiles.append((0, qt, cb0))
                    tiles.append((64, qt, cb1))
                attn_tiles(tiles, hs, 64, 128, "t5", h, False)

            for h in range(4):
                hs = hcount % 2; hcount += 1
                load_head(hs, KTm[h * 128:(h + 1) * 128, :], QTm[h * 128:(h + 1) * 128, :], 128, Vm, h * 129, 129,
                          MBT[h * 32:(h + 1) * 32, :])

                def cbm(os_, qt, h=h):
                    fs = qt % 2

                    A("dve", lambda e: e.reciprocal(out=rcp[1][:, 0:4], in_=Oev[os_][:, :, 128]), r=[("Oev", os_)], w=["rcp1"])

                    def f(e):
                        for j in range(4):
                            e.tensor_scalar(out=fin[fs][:, j, :], in0=Oev[os_][:, j, 0:128],
                                            scalar1=rcp[1][:, j:j + 1], scalar2=None, op0=ALU.mult)
                    A("dve", f, r=[("Oev", os_), "rcp1"], w=[("fin", fs)], par=True)
                    store_fin(fs, 128, 512 + h * 128, qt)
                attn_tiles([(0, qt, cbm) for qt in range(NQ)], hs, 128, 128, "t5", 4 + h, True)

            for h in range(8):
                hs = hcount % 2; hcount += 1
                load_head(hs, KTc[h * 96:(h + 1) * 96, :], QTc[h * 96:(h + 1) * 96, :], 96, Vc, h * 65, 65)

                def cbc(os_, qt, h=h):
                    fs = qt % 2

                    A("dve", lambda e: e.reciprocal(out=rcp[1][:, 0:4], in_=Oev[os_][:, :, 64]), r=[("Oev", os_)], w=["rcp1"])

                    def f(e):
                        for j in range(4):
                            e.tensor_scalar(out=fin[fs][:, j, 0:64], in0=Oev[os_][:, j, 0:64],
                                            scalar1=rcp[1][:, j:j + 1], scalar2=None, op0=ALU.mult)
                    A("dve", f, r=[("Oev", os_), "rcp1"], w=[("fin", fs)], par=True)
                    store_fin(fs, 64, 1024 + h * 64, qt)
                attn_tiles([(0, qt, cbc) for qt in range(NQ)], hs, 96, 64, "causal", 0, False)
        sc.barrier()
        if upto == "P2":
            sc.emit(); es.close(); return nc

        with ExitStack() as st:
            wg = sb("wg", [128, 8, 3072], BF16, st)
            wbr_b = sb("wbr_b", [128, 12, D], BF16, st)
            wout_b = sb("wout_b", [128, 8, D], BF16, st)
            wpost = sb("wpost", [128, D], F32, st)
            with ExitStack() as st2:
                stage = [sb("stg3_%d" % i, [128, 1536], F32, st2) for i in range(2)]
                load_cast(lambda kc, c0, cw: wg[:, kc, c0:c0 + cw],
                          [w_in[l, kc * 128:(kc + 1) * 128, C_G:DIN] for kc in range(8)], 3072, stage, 2, "wst", "wg", 1536)
                load_cast(lambda kc, c0, cw: wbr_b[:, kc, c0:c0 + cw],
                          [wbr[l, kc * 128:(kc + 1) * 128, :] for kc in range(12)], D, stage, 2, "wst", "wbr", D)
                load_cast(lambda kc, c0, cw: wout_b[:, kc, c0:c0 + cw],
                          [wout[l, kc * 128:(kc + 1) * 128, :] for kc in range(8)], D, stage, 2, "wst", "wout", D)
                bcast_row(wpost[:], n_mpo[l:l + 1, :], "wpost")
                sc.barrier()
            hTt = [sb("hTt3_%d" % i, [128, 8, 512], BF16, st) for i in range(2)]
            aTt = [sb("aTt%d" % i, [128, 12, 512], BF16, st) for i in range(2)]
            xt = [sb("xt3_%d" % i, [128, 4, D], F32, st) for i in range(1)]
            mixT = sb("mixT", [128, 8, 512], BF16, st)
            sg = [sb("sg%d" % i, [128, 512], F32, st) for i in range(3)]
            pr = [sb("pr%d" % i, [128, 512], F32, st) for i in range(3)]
            junk = sb("junk3", [128, D], BF16, st)
            ss = sb("ss3", [128, 2 * NT], F32, st)
            tmp = [sb("tmp3_%d" % i, [128, D], F32, st) for i in range(2)]
            gi = 0
            for t in range(NQ):
                s = t % 2
                A("sp", lambda e, s=s, t=t: e.dma_start(
                    out=hTt[s][:], in_=hT_d[:, t * 512:(t + 1) * 512].rearrange("(c p) n -> p c n", p=128)),
                  r=["hT_d"], w=[("hTt", s)], dma=("ldh", s))
                A("sp", lambda e, s=s, t=t: e.dma_start(
                    out=aTt[s][:], in_=attT[:, t * 512:(t + 1) * 512].rearrange("(c p) n -> p c n", p=128)),
                  r=["attT"], w=[("aTt", s)], dma=("lda", s))
                A("sp", lambda e, s=s, t=t: e.dma_start(
                    out=xt[0][:], in_=x_cur[t * 512:(t + 1) * 512, :].rearrange("(j p) d -> p j d", p=128)),
                  r=["xa", "xb"], w=[("xt", 0)], dma=("ldx", 0))
                for oc in range(8):
                    for g in range(3):
                        bg = gi % 6
                        bb = (gi + 1) % 6
                        k3 = (gi // 2) % 3
                        gi += 2

                        def mg(e, bg=bg, g=g, oc=oc):
                            for kc in range(8):
                                ins = e.matmul(PF[bg][:], lhsT=wg[:, kc, g * D + oc * 128:g * D + (oc + 1) * 128],
                                               rhs=hTt[s][:, kc, :], start=(kc == 0), stop=(kc == 7))
                            return ins
                        A("pe", mg, r=["wg", ("hTt", s)], w=[("PF", bg)])

                        def mb_(e, bb=bb, g=g, oc=oc):
                            for c in range(4):
                                ins = e.matmul(PF[bb][:], lhsT=wbr_b[:, g * 4 + c, oc * 128:(oc + 1) * 128],
                                               rhs=aTt[s][:, g * 4 + c, :], start=(c == 0), stop=(c == 3))
                            return ins
                        A("pe", mb_, r=["wbr", ("aTt", s)], w=[("PF", bb)])
                        A("act", lambda e, bg=bg, k3=k3: e.activation(out=sg[k3][:], in_=PF[bg][:], func=AF.Sigmoid),
                          r=[("PF", bg)], w=[("sg", k3)])
                        A("dve", lambda e, bb=bb, k3=k3, g=g: e.tensor_tensor(out=pr[g][:], in0=sg[k3][:], in1=PF[bb][:],
                                                                               op=ALU.mult),
                          r=[("sg", k3), ("PF", bb)], w=[("pr", g)])
                    A("pool", lambda e: e.tensor_tensor(out=pr[0][:], in0=pr[0][:], in1=pr[1][:], op=ALU.add),
                      r=[("pr", 0), ("pr", 1)], w=[("pr", 0)])
                    A("pool", lambda e, oc=oc: e.tensor_tensor(out=mixT[:, oc, :], in0=pr[0][:], in1=pr[2][:], op=ALU.add),
                      r=[("pr", 0), ("pr", 2)], w=[("mixT", oc)])
                for j in range(4):
                    tj = t * 4 + j
                    s2 = tj % 2

                    def mo(e, j=j):
                        for hf in range(2):
                            for kc in range(8):
                                ins = e.matmul(PW[:, hf * 512:(hf + 1) * 512], lhsT=mixT[:, kc, j * 128:(j + 1) * 128],
                                               rhs=wout_b[:, kc, hf * 512:(hf + 1) * 512], start=(kc == 0), stop=(kc == 7))
                        return ins
                    A("pe", mo, r=[("mixT", oc) for oc in range(8)] + ["wout"], w=["PW"])
                    A("act", lambda e, tj=tj: (e.activation(out=junk[:], in_=PW[:], func=AF.Square,
                                                            accum_out=ss[:, 2 * tj:2 * tj + 1]),
                                               rms_rstd(e, ss[:, 2 * tj + 1:2 * tj + 2], ss[:, 2 * tj:2 * tj + 1], D)),
                      r=["PW", "eps"], w=[("ss", tj)])

                    def fz(e, s=s, j=j, tj=tj, s2=s2):
                        return e.scalar_tensor_tensor(out=tmp[s2][:], in0=PW[:], scalar=ss[:, 2 * tj + 1:2 * tj + 2],
                                                      in1=wpost[:], op0=ALU.mult, op1=ALU.mult)
                    A("dve", fz, r=["PW", ("ss", tj), "wpost"], w=[("tmp", s2)])
                    A("pool", lambda e, s=s, j=j, s2=s2: e.tensor_tensor(out=xt[0][:, j, :], in0=xt[0][:, j, :], in1=tmp[s2][:],
                                                                          op=ALU.add),
                      r=[("tmp", s2), ("xt", 0)], w=[("xt", 0)])
                A("pool", lambda e, s=s, t=t: e.dma_start(
                    out=xa[t * 512:(t + 1) * 512, :].rearrange("(j p) d -> p j d", p=128), in_=xt[0][:]),
                  r=[("xt", 0)], w=["xa"], dma=("stx", 0))
        sc.barrier()
        if upto == "P3":
            sc.emit(); es.close(); return nc

        with ExitStack() as st:
            wup_b = sb("wup_b", [128, 8, DFF], BF16, st)
            wdn_b = sb("wdn_b", [128, 32, D], BF16, st)
            wpre = sb("wpre4", [128, D], F32, st)
            wpost = sb("wpost4", [128, D], F32, st)
            with ExitStack() as st2:
                stage = [sb("stg4_%d" % i, [128, 2048], F32, st2) for i in range(2)]
                load_cast(lambda kc, c0, cw: wup_b[:, kc, c0:c0 + cw],
                          [wup[l, kc * 128:(kc + 1) * 128, :] for kc in range(8)], DFF, stage, 2, "wst", "wup", 2048)
                load_cast(lambda kc, c0, cw: wdn_b[:, kc, c0:c0 + cw],
                          [wdn[l, kc * 128:(kc + 1) * 128, :] for kc in range(32)], D, stage, 2, "wst", "wdn", D)
                bcast_row(wpre[:], n_lp[l:l + 1, :], "wpre4")
                bcast_row(wpost[:], n_lpo[l:l + 1, :], "wpost4")
                sc.barrier()
            xt = [sb("xt4_%d" % i, [128, 2, D], F32, st) for i in range(1)]
            hn = [sb("hn4_%d" % i, [128, D], BF16, st) for i in range(2)]
            h2T = sb("h2T", [128, 8, 256], BF16, st)
            uT = sb("uT", [128, 32, 256], BF16, st)
            rl = [sb("rl%d" % i, [128, 256], F32, st) for i in range(2)]
            ss = sb("ss4", [128, 4 * NT], F32, st)
            tmp = [sb("tmp4_%d" % i, [128, D], F32, st) for i in range(1)]
            ui = 0
            for t in range(S // 256):
                s = t % 2
                A("sp", lambda e, s=s, t=t: e.dma_start(
                    out=xt[0][:], in_=xa[t * 256:(t + 1) * 256, :].rearrange("(j p) d -> p j d", p=128)),
                  r=["xa"], w=[("xt", 0)], dma=("ldx", 0))
                for j in range(2):
                    tj = t * 2 + j
                    s2 = tj % 2
                    A("act", lambda e, s=s, j=j, tj=tj: e.activation(out=hn[tj % 2][:], in_=xt[0][:, j, :], func=AF.Square,
                                                                       accum_out=ss[:, 4 * tj:4 * tj + 1]),
                      r=[("xt", 0)], w=[("ssa0", tj), ("hn", tj % 2)])
                    A("act", lambda e, tj=tj: rms_rstd(e, ss[:, 4 * tj + 1:4 * tj + 2], ss[:, 4 * tj:4 * tj + 1], D),
                      r=[("ssa0", tj), "eps"], w=[("ssa", tj)])

                    def nf(e, s=s, j=j, tj=tj, s2=s2):
                        return e.scalar_tensor_tensor(out=hn[s2][:], in0=xt[0][:, j, :], scalar=ss[:, 4 * tj + 1:4 * tj + 2],
                                                      in1=wpre[:], op0=ALU.mult, op1=ALU.mult)
                    A("dve", nf, r=[("xt", 0), ("ssa", tj), "wpre4"], w=[("hn", s2)])

                    def tr8(e, s2=s2):
                        for c in range(8):
                            ins = e.transpose(out=PB[:, c * 128:(c + 1) * 128], in_=hn[s2][:, c * 128:(c + 1) * 128],
                                              identity=identb[:])
                        return ins
                    A("pe", tr8, r=[("hn", s2), "identb"], w=["PB"])
                    A("act", lambda e, j=j: e.copy(out=h2T[:, :, j * 128:(j + 1) * 128],
                                                   in_=PB.rearrange("p (c n) -> p c n", c=8)),
                      r=["PB"], w=["h2T"])
                for fc in range(32):
                    b = ui % 5
                    k2 = ui % 2
                    ui += 1

                    def mu(e, b=b, fc=fc):
                        for kc in range(8):
                            ins = e.matmul(PF[b][:, 0:256], lhsT=wup_b[:, kc, fc * 128:(fc + 1) * 128], rhs=h2T[:, kc, :],
                                           start=(kc == 0), stop=(kc == 7))
                        return ins
                    A("pe", mu, r=["wup", "h2T"], w=[("PF", b)])
                    A("act", lambda e, b=b, k2=k2: e.activation(out=rl[k2][:], in_=PF[b][:, 0:256], func=AF.Relu),
                      r=[("PF", b)], w=[("rl", k2)])
                    A("dve", lambda e, k2=k2, fc=fc: e.tensor_tensor(out=uT[:, fc, :], in0=rl[k2][:], in1=rl[k2][:], op=ALU.mult),
                      r=[("rl", k2)], w=[("uT", fc)])
                for j in range(2):
                    tj = t * 2 + j
                    s2 = tj % 2

                    def md(e, j=j):
                        for hf in range(2):
                            for fc in range(32):
                                ins = e.matmul(PW[:, hf * 512:(hf + 1) * 512], lhsT=uT[:, fc, j * 128:(j + 1) * 128],
                                               rhs=wdn_b[:, fc, hf * 512:(hf + 1) * 512], start=(fc == 0), stop=(fc == 31))
                        return ins
                    A("pe", md, r=[("uT", fc) for fc in range(32)] + ["wdn"], w=["PW"])
                    A("act", lambda e, tj=tj: e.activation(out=tmp[0][:], in_=PW[:], func=AF.Square,
                                                           accum_out=ss[:, 4 * tj + 2:4 * tj + 3]),
                      r=["PW"], w=[("ssb0", tj), ("tmp", 0)])
                    A("act", lambda e, tj=tj: rms_rstd(e, ss[:, 4 * tj + 3:4 * tj + 4], ss[:, 4 * tj + 2:4 * tj + 3], D),
                      r=[("ssb0", tj), "eps"], w=[("ssb", tj)])

                    def fz(e, tj=tj, s2=s2):
                        return e.scalar_tensor_tensor(out=tmp[0][:], in0=PW[:], scalar=ss[:, 4 * tj + 3:4 * tj + 4],
                                                      in1=wpost[:], op0=ALU.mult, op1=ALU.mult)
                    A("dve", fz, r=["PW", ("ssb", tj), "wpost4"], w=[("tmp", 0)])
                    A("pool", lambda e, s=s, j=j, s2=s2: e.tensor_tensor(out=xt[0][:, j, :], in0=xt[0][:, j, :], in1=tmp[0][:],
                                                                          op=ALU.add),
                      r=[("tmp", 0), ("xt", 0)], w=[("xt", 0)])
                A("pool", lambda e, s=s, t=t: e.dma_start(
                    out=x_fin[t * 256:(t + 1) * 256, :].rearrange("(j p) d -> p j d", p=128), in_=xt[0][:]),
                  r=[("xt", 0)], w=["xb"], dma=("stx", 0))
        sc.barrier()
        x_cur = xb

    sc.emit()
    es.close()
    return nc


def _t5_bucket_np(rel):
    n = np.maximum(rel, 0)
    nf = np.maximum(n, 1).astype(np.float32)
    large = 16 + (np.log(nf / np.float32(16)) / np.float32(math.log(64)) * np.float32(16)).astype(np.int32)
    large = np.minimum(large, 31)
    return np.where(n < 16, n, large)


def host_consts(rel_bias):
    kk = np.arange(128)[:, None]
    c = np.arange(SW)[None, :]
    rel = c - kk - 511
    bidx = _t5_bucket_np(rel)
    rb = np.asarray(rel_bias, np.float32)
    strips = np.empty((8, 128, SW), np.float32)
    for h in range(8):
        g = rb[bidx, h]
        strips[h] = np.where(rel >= 0, g, np.float32(NEG))
    c2 = np.arange(1024)[None, :]
    cstrip = np.where(c2 - kk - 511 >= 0, 0.0, NEG).astype(ml_dtypes.bfloat16)
    identb = np.eye(128, dtype=np.float32).astype(ml_dtypes.bfloat16)
    identf = np.eye(128, dtype=np.float32)
    sel = np.zeros((32, 32 * 128), np.float32)
    for n in range(32):
        sel[n, n * 128:(n + 1) * 128] = 1.0
    return dict(strips=strips, cstrip=cstrip, identb=identb, identf=identf, selE=sel.astype(ml_dtypes.bfloat16))


def make_in_maps(x, positions, rel_bias, norm_mix_pre, norm_mix_post, norm_mlp_pre, norm_mlp_post,
                 w_in, diff_lambda, diff_subln, mla_q_norm, mla_w_uq, mla_kv_norm, mla_w_ukv,
                 w_branch, w_out, w_up, w_down, n_cores=8):
    B, S, _ = x.shape
    L = w_in.shape[0]
    f = lambda a: np.ascontiguousarray(np.asarray(a, np.float32))
    shared = dict(
        relb=f(rel_bias), n_mp=f(norm_mix_pre), n_mpo=f(norm_mix_post), n_lp=f(norm_mlp_pre), n_lpo=f(norm_mlp_post),
        w_in=f(w_in), dlam=f(diff_lambda).reshape(L, 256), dsub=f(diff_subln), mqn=f(mla_q_norm), wuq=f(mla_w_uq),
        mkn=f(mla_kv_norm), wukv=f(mla_w_ukv), wbr=f(w_branch).reshape(L, 1536, D), wout=f(w_out), wup=f(w_up),
        wdn=f(w_down))
    shared.update(host_consts(rel_bias))
    maps = []
    for c in range(n_cores):
        b = c % B
        m = dict(shared)
        m["x"] = f(x[b])
        m["pos"] = np.ascontiguousarray(np.asarray(positions[b], np.int32).reshape(S // 128, 128).T)
        maps.append(m)
    return maps


_NC_CACHE = {}


def kernel(**inputs):
    x = np.asarray(inputs["x"])
    B, S, _ = x.shape
    L = np.asarray(inputs["w_in"]).shape[0]
    key = (S, L)
    if key not in _NC_CACHE:
        _NC_CACHE[key] = build(S, L)
    nc = _NC_CACHE[key]
    maps = make_in_maps(**inputs, n_cores=B)
    res = run_bass_kernel_spmd(nc, maps, core_ids=list(range(B)))
    out = np.stack([np.asarray(res.results[b]["y"], np.float32) for b in range(B)], axis=0)
    return out
```

```python
import math
from contextlib import ExitStack

import numpy as np
import ml_dtypes

import concourse.bass as bass
import concourse.mybir as mybir
from concourse.bass_utils import run_bass_kernel_spmd

F32 = mybir.dt.float32
BF16 = mybir.dt.bfloat16
I32 = mybir.dt.int32
AF = mybir.ActivationFunctionType
ALU = mybir.AluOpType
AX = mybir.AxisListType

D = 1024
DIN = 6560
DFF = 4096
EPS = 1e-6
NEG = -30000.0
SW = 1920
C_DQ, C_DK, C_DV, C_MQ, C_MK, C_MV, C_CQ, C_CKV, C_KPE, C_G = (
    0, 512, 1024, 1536, 2048, 2560, 3072, 3328, 3456, 3488)
INVF = [1.0, 0.5623413324356079, 0.3162277638912201, 0.17782793939113617,
        0.10000000149011612, 0.05623413249850273, 0.03162277489900589,
        0.017782794311642647, 0.009999999776482582, 0.005623413249850273,
        0.003162277629598975, 0.0017782794311642647, 0.0010000000474974513,
        0.000562341301701963, 0.0003162277571391314, 0.00017782794020604342]


class _Rec:
    def __init__(self):
        self.calls = []

    def __getattr__(self, name):
        def f(*a, **k):
            self.calls.append((name, a, k))
            return None
        return f


class Sched:
    ENG = ("sp", "act", "pe", "dve", "pool")

    def __init__(self, nc):
        self.nc = nc
        self.ops = []

    def add(self, eng, fn, r=(), w=(), dma=None, par=False):
        rec = _Rec()
        fn(rec)
        assert rec.calls, "op emitted nothing"
        if dma is not None:
            assert len(rec.calls) == 1
        if eng != "pe" and len(rec.calls) > 1 and not par:
            for c in rec.calls:
                self.ops.append([eng, [c], tuple(r), tuple(w), dma])
            return
        self.ops.append([eng, rec.calls, tuple(r), tuple(w), dma])

    def barrier(self):
        self.ops.append(["barrier"])

    def emit(self):
        import os
        if os.environ.get("KTRUNC"):
            self.ops = self.ops[:int(os.environ["KTRUNC"])]
        nc, ops = self.nc, self.ops
        n = len(ops)
        lastw, rd_eng, rd_dma = {}, {}, {}
        deps = [None] * n
        needs = [False] * n
        for i, op in enumerate(ops):
            if op[0] == "barrier":
                continue
            eng, fn, r, w, dma = op
            d = set()
            for k in r:
                if k in lastw:
                    d.add(lastw[k])
            for k in w:
                if k in lastw:
                    d.add(lastw[k])
                d.update(rd_eng.get(k, {}).values())
                d.update(rd_dma.get(k, ()))
            d.discard(i)
            for k in r:
                if dma is None:
                    rd_eng.setdefault(k, {})[eng] = i
                else:
                    rd_dma.setdefault(k, []).append(i)
            for k in w:
                lastw[k] = i
                rd_eng[k] = {}
                rd_dma[k] = []
            dd = []
            for j in sorted(d):
                oj = ops[j]
                if oj[4] is None and dma is None and oj[0] == eng == "pe":
                    continue
                dd.append(j)
                if oj[4] is None:
                    needs[j] = True
            deps[i] = dd
        last_on = {}
        for i, op in enumerate(ops):
            if op[0] == "barrier":
                for j in last_on.values():
                    needs[j] = True
            elif op[4] is None:
                last_on[op[0]] = i
        for j in last_on.values():
            needs[j] = True
        RING = {"sp": 6, "pool": 5}
        cnt = {e: 0 for e in self.ENG}
        dcnt = {}
        ndma = {}
        ring_last = {}
        val = [None] * n
        for i, op in enumerate(ops):
            if op[0] == "barrier":
                op.append((dict(cnt), dict(dcnt)))
                continue
            if op[4] is not None:
                q = op[0]
                k = (q, ndma.get(q, 0) % RING[q])
                ndma[q] = ndma.get(q, 0) + 1
                if k in ring_last:
                    deps[i] = list(deps[i]) + [ring_last[k]]
                ring_last[k] = i
                dcnt[k] = dcnt.get(k, 0) + 16
                val[i] = (("dma", k), dcnt[k])
            elif needs[i]:
                cnt[op[0]] += 1
                val[i] = (("eng", op[0]), cnt[op[0]])
        final = (dict(cnt), dict(dcnt))
        keys = [("eng", e) for e in self.ENG] + [("dma", k) for k in dcnt]
        assert len(keys) <= 16, len(keys)
        with ExitStack() as st:
            sems = {}
            for idx, k in enumerate(keys):
                sems[k] = st.enter_context(nc.semaphore("s%d" % idx))
            with nc.Block() as block:
                def mk(E):
                    def body(e):
                        waited = {}

                        def wait(sk, v):
                            if v > 0 and waited.get(sk, 0) < v:
                                e.wait_ge(sems[sk], v)
                                waited[sk] = v

                        def wait_all(state):
                            cn, dc = state
                            for en, v in cn.items():
                                wait(("eng", en), v)
                            for k, v in dc.items():
                                wait(("dma", k), v)

                        for i, op in enumerate(ops):
                            if op[0] == "barrier":
                                wait_all(op[-1])
                                continue
                            if op[0] != E:
                                continue
                            for j in deps[i]:
                                wait(*val[j])
                            ins = None
                            for (nm, a, k) in op[1]:
                                ins = getattr(e, nm)(*a, **k)
                            if val[i] is not None:
                                ins.then_inc(sems[val[i][0]], 16 if val[i][0][0] == "dma" else 1)
                        wait_all(final)
                    return body
                block.sync(mk("sp"))
                block.scalar(mk("act"))
                block.tensor(mk("pe"))
                block.vector(mk("dve"))
                block.gpsimd(mk("pool"))


def build(S, L, dbg=False, upto=None):
    NT = S // 128
    NQ = S // 512
    NB = S // 256
    nc = bass.Bass("TRN2", target_bir_lowering=False)
    sc = Sched(nc)

    def din(name, shape, dt=F32):
        return nc.dram_tensor(name, list(shape), dt, kind="ExternalInput").ap()

    def dscr(name, shape, dt):
        if dbg:
            return nc.dram_tensor(name, list(shape), dt, kind="ExternalOutput").ap()
        return nc.dram_tensor(name, list(shape), dt).ap()

    x_in = din("x", [S, D])
    pos_in = din("pos", [128, NT], I32)
    relb = din("relb", [32, 8])
    n_mp = din("n_mp", [L, D]); n_mpo = din("n_mpo", [L, D])
    n_lp = din("n_lp", [L, D]); n_lpo = din("n_lpo", [L, D])
    w_in = din("w_in", [L, D, DIN])
    dlam = din("dlam", [L, 256]); dsub = din("dsub", [L, 128])
    mqn = din("mqn", [L, 256]); wuq = din("wuq", [L, 256, 768])
    mkn = din("mkn", [L, 128]); wukv = din("wukv", [L, 128, 1024])
    wbr = din("wbr", [L, 1536, D]); wout = din("wout", [L, D, D])
    wup = din("wup", [L, D, DFF]); wdn = din("wdn", [L, DFF, D])
    strips_in = din("strips", [8, 128, SW])
    identb_in = din("identb", [128, 128], BF16)
    identf_in = din("identf", [128, 128])
    sel_in = din("selE", [32, 32 * 128], BF16)
    cstrip_in = din("cstrip", [128, 1024], BF16)
    y_out = nc.dram_tensor("y", [S, D], F32, kind="ExternalOutput").ap()

    hT_d = dscr("hT_d", [D, S], BF16)
    QTd = dscr("QTd", [512, S], BF16); KTd = dscr("KTd", [512, S], BF16)
    QTm = dscr("QTm", [512, S], BF16); KTm = dscr("KTm", [512, S], BF16)
    Vd = dscr("Vd", [S, 4 * 129], BF16); Vm = dscr("Vm", [S, 4 * 129], BF16)
    MBT = dscr("MBT", [128, S], BF16)
    QTc = dscr("QTc", [768, S], BF16); KTc = dscr("KTc", [768, S], BF16)
    Vc = dscr("Vc", [S, 8 * 65], BF16)
    attT = dscr("attT", [1536, S], BF16)
    xa = dscr("xa", [S, D], F32)
    xb = dscr("xb", [S, D], F32)

    es = ExitStack()

    uid = [0]

    def sb(name, shape, dt, st=None):
        uid[0] += 1
        return (st or es).enter_context(nc.sbuf_tensor("sb_%s_%d" % (name, uid[0]), list(shape), dt))

    def ps(name, shape, dt, st=None):
        return (st or es).enter_context(nc.psum_tensor("ps_" + name, list(shape), dt))

    PF = [ps("pf%d" % i, [128, 512], F32) for i in range(6)]
    PW = ps("pw", [128, 1024], F32)
    PB = PF[5][:].bitcast(BF16)

    identb = sb("identb", [128, 128], BF16)
    identf = sb("identf", [128, 128], F32)
    cosT = sb("cosT", [128, NT, 16], F32)
    sinT = sb("sinT", [128, NT, 16], F32)
    lamt = sb("lamt", [128, 8], F32)
    eps_t = sb("eps_t", [128, 1], F32)

    A = sc.add

    with ExitStack() as st:
        posi = sb("posi", [128, NT], I32, st)
        posf = sb("posf", [128, NT], F32, st)
        ang = sb("ang", [128, NT, 16], F32, st)
        A("sp", lambda e: e.dma_start(out=identb[:], in_=identb_in[:, :]), w=["identb"], dma="c0")
        A("sp", lambda e: e.dma_start(out=identf[:], in_=identf_in[:, :]), w=["identf"], dma="c1")
        A("sp", lambda e: e.dma_start(out=posi[:], in_=pos_in[:, :]), w=["posi"], dma="c2")
        A("dve", lambda e: e.tensor_copy(out=posf[:], in_=posi[:]), r=["posi"], w=["posf"])
        A("dve", lambda e: e.memset(eps_t[:], EPS), w=["eps"])
        for i in range(16):
            A("dve", lambda e, i=i: e.tensor_scalar(out=ang[:, :, i], in0=posf[:], scalar1=float(INVF[i]),
                                                    scalar2=None, op0=ALU.mult), r=["posf"], w=[("ang", i)])
        allang = [("ang", i) for i in range(16)]
        ki = sb("ki", [128, NT, 16], I32, st)
        kf = sb("kf", [128, NT, 16], F32, st)
        a2 = sb("a2", [128, NT, 16], F32, st)
        TWO_PI = 2.0 * math.pi
        for dst, shift, key in ((cosT, 0.5 * math.pi, "cosT"), (sinT, 0.0, "sinT")):
            def f1(e, dst=dst, shift=shift):
                e.tensor_scalar(out=a2[:], in0=ang[:], scalar1=float(shift), scalar2=None, op0=ALU.add)
                e.tensor_scalar(out=kf[:], in0=a2[:], scalar1=float(1.0 / TWO_PI), scalar2=None, op0=ALU.mult)
                e.tensor_copy(out=ki[:], in_=kf[:])
                e.tensor_copy(out=kf[:], in_=ki[:])
                e.scalar_tensor_tensor(out=dst[:], in0=kf[:], scalar=float(-TWO_PI), in1=a2[:], op0=ALU.mult, op1=ALU.add)
                e.tensor_scalar(out=kf[:], in0=dst[:], scalar1=float(math.pi), scalar2=float(-TWO_PI),
                                op0=ALU.is_gt, op1=ALU.mult)
                e.tensor_tensor(out=dst[:], in0=dst[:], in1=kf[:], op=ALU.add)
                return e.tensor_scalar(out=dst[:], in0=dst[:], scalar1=float(-math.pi), scalar2=float(math.pi),
                                       op0=ALU.max, op1=ALU.min)
            A("dve", f1, r=allang, w=[key + "0", "rr_tmp"])
            A("act", lambda e, dst=dst: e.activation(out=dst[:], in_=dst[:], func=AF.Sin),
              r=[key + "0"], w=[key])
    sc.barrier()
    if upto == "P0":
        A("pool", lambda e: e.dma_start(out=xa[0:128, 0:16], in_=cosT[:, 0, :]), r=["cosT"], dma="dbg0")
        A("pool", lambda e: e.dma_start(out=xa[0:128, 16:32], in_=sinT[:, 0, :]), r=["sinT"], dma="dbg1")
        A("pool", lambda e: e.dma_start(out=xa[128:256, 0:16], in_=cosT[:, NT - 1, :]), r=["cosT"], dma="dbg2")
        sc.emit(); es.close(); return nc

    def load_cast(dst_ap_fn, src_rows, ncols, stage, nslot, tag, key_w, col_chunk, cast_engs=("pool", "dve")):
        cnt = 0
        for kc, src in enumerate(src_rows):
            for c0 in range(0, ncols, col_chunk):
                cw = min(col_chunk, ncols - c0)
                s = cnt % nslot
                A("sp", lambda e, s=s, src=src, c0=c0, cw=cw: e.dma_start(out=stage[s][:, 0:cw], in_=src[:, c0:c0 + cw]),
                  w=[(tag, s)], dma=(tag, s))
                ce = cast_engs[cnt % len(cast_engs)]
                A(ce, lambda e, s=s, kc=kc, c0=c0, cw=cw: e.tensor_copy(out=dst_ap_fn(kc, c0, cw), in_=stage[s][:, 0:cw]),
                  r=[(tag, s)], w=[key_w])
                cnt += 1

    def rms_rstd(e, out_ap, ss_ap, n):
        e.activation(out=out_ap, in_=ss_ap, func=AF.Ln, scale=1.0 / n, bias=eps_t[0:out_ap.shape[0], 0:1])
        return e.activation(out=out_ap, in_=out_ap, func=AF.Exp, scale=-0.5)

    def bcast_row(dst, src_row_ap, key):
        A("sp", lambda e: e.dma_start(out=dst, in_=src_row_ap.partition_broadcast(128)), w=[key], dma=("bc", key))

    x_cur = x_in
    for l in range(L):
        lam_init = 0.8 - 0.6 * math.exp(-0.3 * l)
        last = (l == L - 1)
        x_fin = y_out if last else xb

        with ExitStack() as st:
            w1 = sb("w1", [128, 8, C_G], BF16, st)
            wuq_b = sb("wuq_b", [128, 2, 768], BF16, st)
            wukv_b = sb("wukv_b", [128, 1024], BF16, st)
            wpre = sb("wpre", [128, D], F32, st)
            mqn_b = sb("mqn_b", [128, 256], F32, st)
            mkn_b = sb("mkn_b", [128, 128], F32, st)
            kmT = sb("kmT", [128, 4, 32], F32, st)
            vmk = sb("vmk", [128, NB, 32], F32, st)
            stage = [sb("stg%d" % i, [128, 872], F32, st) for i in range(2)]
            xt = [sb("xt%d" % i, [128, 4, D], F32, st) for i in range(2)]
            hTt = [sb("hTt%d" % i, [128, 8, 512], BF16, st) for i in range(2)]
            hn = [sb("hn%d" % i, [128, D], BF16, st) for i in range(2)]
            junk = sb("junk", [128, 384], BF16, st)
            ss = sb("ss", [128, NT], F32, st)
            rstd = sb("rstd", [128, NT], F32, st)
            ostg = [sb("ostg%d" % i, [128, 512], BF16, st) for i in range(4)]
            qf = [sb("qf%d" % i, [128, 512], F32, st) for i in range(2)]
            gm = [sb("gm%d" % i, [128, 32], F32, st) for i in range(2)]
            m8 = [sb("m8%d" % i, [128, 8], F32, st) for i in range(2)]
            mbs = [sb("mbs%d" % i, [128, 32], F32, st) for i in range(2)]
            mbTs = [sb("mbTs%d" % i, [32, 512], BF16, st) for i in range(2)]
            vst = [sb("vst%d" % i, [128, 4, 4, 129], BF16, st) for i in range(2)]
            vcst = [sb("vcst%d" % i, [128, 4, 8, 65], BF16, st) for i in range(2)]
            lat = [sb("lat%d" % i, [128, 416], F32, st) for i in range(2)]
            lss = sb("lss", [128, 4 * NT], F32, st)
            latn = [sb("latn%d" % i, [128, 384], BF16, st) for i in range(2)]
            latT = [sb("latT%d" % i, [128, 3, 128], BF16, st) for i in range(2)]
            qcb = [sb("qcb%d" % i, [128, 8, 96], BF16, st) for i in range(2)]
            kcb = [sb("kcb%d" % i, [128, 8, 96], BF16, st) for i in range(2)]
            kr = [sb("kr%d" % i, [128, 32], F32, st) for i in range(2)]
            rt = [sb("rt%d" % i, [128, 8, 16], F32, st) for i in range(4)]
            qcTs = [sb("qcTs%d" % i, [128, 6, 512], BF16, st) for i in range(1)]
            kcTs = [sb("kcTs%d" % i, [128, 6, 512], BF16, st) for i in range(1)]

            load_cast(lambda kc, c0, cw: w1[:, kc, c0:c0 + cw],
                      [w_in[l, kc * 128:(kc + 1) * 128, :] for kc in range(8)], C_G, stage, 2, "wst", "w1", 872)
            load_cast(lambda kc, c0, cw: wuq_b[:, kc, c0:c0 + cw],
                      [wuq[l, kc * 128:(kc + 1) * 128, :] for kc in range(2)], 768, stage, 2, "wst", "wuq", 768)
            load_cast(lambda kc, c0, cw: wukv_b[:, c0:c0 + cw], [wukv[l, :, :]], 1024, stage, 2, "wst", "wukv", 872)
            bcast_row(wpre[:], n_mp[l:l + 1, :], "wpre")
            bcast_row(mqn_b[:], mqn[l:l + 1, :], "mqn")
            bcast_row(mkn_b[:], mkn[l:l + 1, :], "mkn")
            A("dve", lambda e: e.memset(kmT[:], 0.0), w=["kmT"])
            A("pool", lambda e: e.memset(vmk[:], -1e30), w=["vmk0"])

            def vmf(e):
                for own in range(NB):
                    if own > 0:
                        e.memset(vmk[:, own, 0:own], 0.0)
                    e.memset(vmk[:, own, own:own + 1], 1e30)
            A("pool", vmf, r=["vmk0"], w=["vmk"], par=True)
            for i in range(2):
                A("pool", lambda e, i=i: e.memset(vst[i][:], 1.0), w=[("vst", i)])
                A("pool", lambda e, i=i: e.memset(vcst[i][:], 1.0), w=[("vcst", i)])

            pfi = [0]

            def nextpf():
                b = pfi[0] % 5
                pfi[0] += 1
                return b

            for t in range(NQ):
                s = t % 2
                A("sp", lambda e, s=s, t=t: e.dma_start(
                    out=xt[s][:], in_=x_cur[t * 512:(t + 1) * 512, :].rearrange("(j p) d -> p j d", p=128)),
                  w=[("xt", s)], dma=("ldx", s))
                for j in range(4):
                    tj = t * 4 + j
                    s2 = tj % 2
                    A("act", lambda e, s=s, j=j, tj=tj: e.activation(out=hn[tj % 2][:], in_=xt[s][:, j, :], func=AF.Square,
                                                                       accum_out=ss[:, tj:tj + 1]),
                      r=[("xt", s)], w=[("ss", tj), ("hn", tj % 2)])
                    A("act", lambda e, tj=tj: rms_rstd(e, rstd[:, tj:tj + 1], ss[:, tj:tj + 1], D),
                      r=[("ss", tj), "eps"], w=[("rstd", tj)])
                    A("dve", lambda e, s=s, j=j, tj=tj, s2=s2: e.scalar_tensor_tensor(
                        out=hn[s2][:], in0=xt[s][:, j, :], scalar=rstd[:, tj:tj + 1], in1=wpre[:],
                        op0=ALU.mult, op1=ALU.mult), r=[("xt", s), ("rstd", tj), "wpre"], w=[("hn", s2)])

                    def tr8(e, s2=s2):
                        for c in range(8):
                            ins = e.transpose(out=PB[:, c * 128:(c + 1) * 128], in_=hn[s2][:, c * 128:(c + 1) * 128],
                                              identity=identb[:])
                        return ins
                    A("pe", tr8, r=[("hn", s2), "identb"], w=["PB"])
                    A("act", lambda e, s=s, j=j: e.copy(out=hTt[s][:, :, j * 128:(j + 1) * 128],
                                                        in_=PB.rearrange("p (c n) -> p c n", c=8)),
                      r=["PB"], w=[("hTt", s)])
                A("pool", lambda e, s=s, t=t: e.dma_start(
                    out=hT_d[:, t * 512:(t + 1) * 512].rearrange("(c p) n -> p c n", p=128), in_=hTt[s][:]),
                  r=[("hTt", s)], w=["hT_d"], dma=("sthT", s))

                def fm_chunk(co, dst, row0, scale, kind, h, oi):
                    b = nextpf()

                    def mm(e, b=b, co=co):
                        for kc in range(8):
                            ins = e.matmul(PF[b][:], lhsT=w1[:, kc, co:co + 128], rhs=hTt[s][:, kc, :],
                                           start=(kc == 0), stop=(kc == 7))
                        return ins
                    A("pe", mm, r=["w1", ("hTt", s)], w=[("PF", b)])
                    so = oi % 4
                    A("act", lambda e, b=b, so=so: e.mul(out=ostg[so][:], in_=PF[b][:], mul=float(scale)),
                      r=[("PF", b)], w=[("ostg", so), ("PFr", b)])
                    A("pool", lambda e, so=so: e.dma_start(out=dst[row0:row0 + 128, t * 512:(t + 1) * 512], in_=ostg[so][:]),
                      r=[("ostg", so)], w=[dst.tensor.name], dma=("sto", so))
                    if kind == "mk":
                        A("dve", lambda e, b=b: e.tensor_reduce(
                            out=kmT[:, h, 2 * t:2 * t + 2], in_=PF[b][:].rearrange("p (a n) -> p a n", a=2),
                            axis=AX.X, op=ALU.add), r=[("PF", b)], w=["kmT", ("PFr", b)])
                    if kind == "mq":
                        sq = h % 2
                        A("dve", lambda e, b=b, sq=sq: e.tensor_copy(out=qf[sq][:], in_=PF[b][:]),
                          r=[("PF", b)], w=[("qf", sq), ("PFr", b)])
                        for j in range(4):
                            own = 2 * t + j // 2
                            sg = (h * 4 + j) % 2
                            bg = nextpf()
                            A("pe", lambda e, bg=bg, sq=sq, j=j: e.matmul(
                                PF[bg][:, 0:32], lhsT=qf[sq][:, j * 128:(j + 1) * 128], rhs=kmT[:, h, :],
                                start=True, stop=True), r=[("qf", sq), "kmT"], w=[("PF", bg)])

                            def sel(e, bg=bg, sg=sg, own=own):
                                e.tensor_tensor(out=gm[sg][:], in0=PF[bg][:, 0:32], in1=vmk[:, own, :], op=ALU.add)
                                e.max(out=m8[sg][:], in_=gm[sg][:])
                                e.tensor_scalar(out=m8[sg][:, 3:4], in0=m8[sg][:, 3:4], scalar1=-1e29, scalar2=None,
                                                op0=ALU.max)
                                return e.tensor_scalar(out=mbs[sg][:], in0=gm[sg][:], scalar1=m8[sg][:, 3:4], scalar2=NEG,
                                                       op0=ALU.is_lt, op1=ALU.mult)
                            A("dve", sel, r=[("PF", bg), "vmk"], w=[("mbs", sg)])
                            bt = nextpf()
                            A("pe", lambda e, bt=bt, sg=sg: e.transpose(out=PF[bt][0:32, 0:128], in_=mbs[sg][:],
                                                                        identity=identf[:]),
                              r=[("mbs", sg), "identf"], w=[("PF", bt)])
                            sm = h % 2
                            A("act", lambda e, bt=bt, sm=sm, j=j: e.copy(out=mbTs[sm][:, j * 128:(j + 1) * 128],
                                                                         in_=PF[bt][0:32, 0:128]),
                              r=[("PF", bt)], w=[("mbTs", sm)])
                        A("pool", lambda e, sm=sm: e.dma_start(out=MBT[h * 32:(h + 1) * 32, t * 512:(t + 1) * 512],
                                                              in_=mbTs[sm][:]),
                          r=[("mbTs", sm)], w=["MBT"], dma=("stmb", sm))

                oi = 0
                for h in range(4):
                    fm_chunk(C_DQ + h * 128, QTd, h * 128, 0.125, "dq", h, oi); oi += 1
                    fm_chunk(C_DK + h * 128, KTd, h * 128, 1.0, "dk", h, oi); oi += 1
                for h in range(4):
                    fm_chunk(C_MK + h * 128, KTm, h * 128, 1.0, "mk", h, oi); oi += 1
                for h in range(4):
                    fm_chunk(C_MQ + h * 128, QTm, h * 128, 1.0 / math.sqrt(128.0), "mq", h, oi); oi += 1

                for gi, (co, dstV) in enumerate(((C_DV, Vd), (C_MV, Vm))):
                    sv = gi
                    for j in range(4):
                        b = nextpf()

                        def mmv(e, b=b, co=co, j=j):
                            for kc in range(8):
                                ins = e.matmul(PF[b][:], lhsT=hTt[s][:, kc, j * 128:(j + 1) * 128],
                                               rhs=w1[:, kc, co:co + 512], start=(kc == 0), stop=(kc == 7))
                            return ins
                        A("pe", mmv, r=["w1", ("hTt", s)], w=[("PF", b)])
                        A("act", lambda e, b=b, sv=sv, j=j: e.copy(out=vst[sv][:, j, :, 0:128],
                                                                   in_=PF[b][:].rearrange("p (h e) -> p h e", h=4)),
                          r=[("PF", b)], w=[("vst", sv)])
                    A("pool", lambda e, sv=sv, dstV=dstV: e.dma_start(
                        out=dstV[t * 512:(t + 1) * 512, :].rearrange("(j p) f -> p j f", p=128),
                        in_=vst[sv][:].rearrange("p j h e -> p j (h e)")),
                      r=[("vst", sv)], w=[dstV.tensor.name], dma=("stv", sv))

                sT = 0
                sV = t % 2
                for j in range(4):
                    tj = t * 4 + j
                    sl = tj % 2
                    b = nextpf()

                    def mml(e, b=b, j=j):
                        for kc in range(8):
                            ins = e.matmul(PF[b][:, 0:416], lhsT=hTt[s][:, kc, j * 128:(j + 1) * 128],
                                           rhs=w1[:, kc, C_CQ:C_CQ + 416], start=(kc == 0), stop=(kc == 7))
                        return ins
                    A("pe", mml, r=["w1", ("hTt", s)], w=[("PF", b)])
                    A("dve", lambda e, b=b, sl=sl: e.tensor_copy(out=lat[sl][:], in_=PF[b][:, 0:416]),
                      r=[("PF", b)], w=[("lat", sl)])

                    def lnorm(e, sl=sl, tj=tj):
                        e.activation(out=junk[:, 0:256], in_=lat[sl][:, 0:256], func=AF.Square,
                                     accum_out=lss[:, 4 * tj:4 * tj + 1])
                        e.activation(out=junk[:, 256:384], in_=lat[sl][:, 256:384], func=AF.Square,
                                     accum_out=lss[:, 4 * tj + 1:4 * tj + 2])
                    A("act", lnorm, r=[("lat", sl)], w=[("lss0", tj)], par=True)

                    def lnormb(e, tj=tj):
                        e.activation(out=lss[:, 4 * tj + 2:4 * tj + 3], in_=lss[:, 4 * tj:4 * tj + 1], func=AF.Ln,
                                     scale=1.0 / 256, bias=eps_t[:, 0:1])
                        e.activation(out=lss[:, 4 * tj + 3:4 * tj + 4], in_=lss[:, 4 * tj + 1:4 * tj + 2], func=AF.Ln,
                                     scale=1.0 / 128, bias=eps_t[:, 0:1])
                    A("act", lnormb, r=[("lss0", tj), "eps"], w=[("lss1", tj)], par=True)
                    A("act", lambda e, tj=tj: e.activation(out=lss[:, 4 * tj + 2:4 * tj + 4], in_=lss[:, 4 * tj + 2:4 * tj + 4],
                                                           func=AF.Exp, scale=-0.5),
                      r=[("lss1", tj)], w=[("lss", tj)])

                    def lnorm2(e, sl=sl, tj=tj):
                        e.scalar_tensor_tensor(out=latn[sl][:, 0:256], in0=lat[sl][:, 0:256],
                                               scalar=lss[:, 4 * tj + 2:4 * tj + 3], in1=mqn_b[:],
                                               op0=ALU.mult, op1=ALU.mult)
                        return e.scalar_tensor_tensor(out=latn[sl][:, 256:384], in0=lat[sl][:, 256:384],
                                                      scalar=lss[:, 4 * tj + 3:4 * tj + 4], in1=mkn_b[:],
                                                      op0=ALU.mult, op1=ALU.mult)
                    A("dve", lnorm2, r=[("lat", sl), ("lss", tj), "mqn", "mkn"], w=[("latn", sl)], par=True)

                    def tr3(e, sl=sl):
                        for c in range(3):
                            ins = e.transpose(out=PB[:, c * 128:(c + 1) * 128], in_=latn[sl][:, c * 128:(c + 1) * 128],
                                              identity=identb[:])
                        return ins
                    A("pe", tr3, r=[("latn", sl), "identb"], w=["PB"])
                    A("act", lambda e, sl=sl: e.copy(out=latT[sl][:], in_=PB[:, 0:384].rearrange("p (c n) -> p c n", c=3)),
                      r=["PB"], w=[("latT", sl)])

                    def mmq(e, sl=sl):
                        for hf in range(2):
                            for c in range(2):
                                ins = e.matmul(PW[:, hf * 512:hf * 512 + 384], lhsT=latT[sl][:, c, :],
                                               rhs=wuq_b[:, c, hf * 384:(hf + 1) * 384], start=(c == 0), stop=(c == 1))
                        return ins
                    A("pe", mmq, r=[("latT", sl), "wuq"], w=["PW"])
                    cs = cosT[:, tj, :]
                    sn = sinT[:, tj, :]

                    def ropeq(e, sl=sl, cs=cs, sn=sn):
                        for hf in range(2):
                            v = PW[:, hf * 512:hf * 512 + 384].rearrange("p (h e) -> p h e", h=4)
                            x1 = v[:, :, 64:80]
                            x2 = v[:, :, 80:96]
                            cb = cs.unsqueeze(1).broadcast_to([128, 4, 16])
                            sb_ = sn.unsqueeze(1).broadcast_to([128, 4, 16])
                            hs = slice(hf * 4, hf * 4 + 4)
                            e.tensor_tensor(out=rt[0][:, hs, :], in0=x1, in1=cb, op=ALU.mult)
                            e.tensor_tensor(out=rt[1][:, hs, :], in0=x2, in1=sb_, op=ALU.mult)
                            e.tensor_tensor(out=rt[2][:, hs, :], in0=x2, in1=cb, op=ALU.mult)
                            ins = e.tensor_tensor(out=rt[3][:, hs, :], in0=x1, in1=sb_, op=ALU.mult)
                    A("dve", ropeq, r=["PW", "cosT", "sinT"], w=["rt", "PWr"], par=True)

                    def ropeq2(e, sl=sl):
                        e.tensor_tensor(out=qcb[sl][:, :, 64:80], in0=rt[0][:], in1=rt[1][:], op=ALU.subtract)
                        return e.tensor_tensor(out=qcb[sl][:, :, 80:96], in0=rt[2][:], in1=rt[3][:], op=ALU.add)
                    A("dve", ropeq2, r=["rt"], w=[("qcb_r", sl)], par=True)

                    def qnope(e, sl=sl):
                        for hf in range(2):
                            v = PW[:, hf * 512:hf * 512 + 384].rearrange("p (h e) -> p h e", h=4)
                            ins = e.copy(out=qcb[sl][:, hf * 4:hf * 4 + 4, 0:64], in_=v[:, :, 0:64])
                        return ins
                    A("act", qnope, r=["PW"], w=[("qcb_n", sl), "PWr"], par=True)

                    def trq(e, sl=sl):
                        qv = qcb[sl][:].rearrange("p h e -> p (h e)")
                        for c in range(6):
                            ins = e.transpose(out=PB[:, c * 128:(c + 1) * 128], in_=qv[:, c * 128:(c + 1) * 128],
                                              identity=identb[:])
                        return ins
                    A("pe", trq, r=[("qcb_r", sl), ("qcb_n", sl), "identb"], w=["PB"])
                    A("act", lambda e, j=j: e.mul(out=qcTs[sT][:, :, j * 128:(j + 1) * 128],
                                                  in_=PB[:, 0:768].rearrange("p (c n) -> p c n", c=6),
                                                  mul=float(1.0 / math.sqrt(96.0))),
                      r=["PB"], w=[("qcTs", sT)])

                    def mmkv(e, sl=sl):
                        for hf in range(2):
                            ins = e.matmul(PW[:, hf * 512:(hf + 1) * 512], lhsT=latT[sl][:, 2, :],
                                           rhs=wukv_b[:, hf * 512:(hf + 1) * 512], start=True, stop=True)
                        return ins
                    A("pe", mmkv, r=[("latT", sl), "wukv"], w=["PW"])

                    def ropek(e, sl=sl, cs=cs, sn=sn):
                        x1 = lat[sl][:, 384:400]
                        x2 = lat[sl][:, 400:416]
                        e.tensor_tensor(out=rt[0][:, 0, :], in0=x1, in1=cs, op=ALU.mult)
                        e.tensor_tensor(out=rt[1][:, 0, :], in0=x2, in1=sn, op=ALU.mult)
                        e.tensor_tensor(out=rt[2][:, 0, :], in0=x2, in1=cs, op=ALU.mult)
                        e.tensor_tensor(out=rt[3][:, 0, :], in0=x1, in1=sn, op=ALU.mult)
                    A("dve", ropek, r=[("lat", sl), "cosT", "sinT"], w=["rt"], par=True)

                    def ropek2(e, sl=sl):
                        e.tensor_tensor(out=kr[sl][:, 0:16], in0=rt[0][:, 0, :], in1=rt[1][:, 0, :], op=ALU.subtract)
                        e.tensor_tensor(out=kr[sl][:, 16:32], in0=rt[2][:, 0, :], in1=rt[3][:, 0, :], op=ALU.add)
                    A("dve", ropek2, r=["rt"], w=[("kr", sl)], par=True)
                    A("dve", lambda e, sl=sl: e.tensor_copy(out=kcb[sl][:, :, 64:96],
                                                            in_=kr[sl][:].unsqueeze(1).broadcast_to([128, 8, 32])),
                      r=[("kr", sl)], w=[("kcb_r", sl)])

                    def kvcopy(e, sl=sl, j=j):
                        v = PW[:].rearrange("p (h e) -> p h e", h=8)
                        e.copy(out=kcb[sl][:, :, 0:64], in_=v[:, :, 0:64])
                        return e.copy(out=vcst[sV][:, j, :, 0:64], in_=v[:, :, 64:128])
                    A("act", kvcopy, r=["PW"], w=[("kcb_n", sl), ("vcst", sV)], par=True)

                    def trk(e, sl=sl):
                        kv_ = kcb[sl][:].rearrange("p h e -> p (h e)")
                        for c in range(6):
                            ins = e.transpose(out=PB[:, c * 128:(c + 1) * 128], in_=kv_[:, c * 128:(c + 1) * 128],
                                              identity=identb[:])
                        return ins
                    A("pe", trk, r=[("kcb_r", sl), ("kcb_n", sl), "identb"], w=["PB"])
                    A("act", lambda e, j=j: e.copy(out=kcTs[sT][:, :, j * 128:(j + 1) * 128],
                                                   in_=PB[:, 0:768].rearrange("p (c n) -> p c n", c=6)),
                      r=["PB"], w=[("kcTs", sT)])
                A("pool", lambda e, t=t, sT=sT: e.dma_start(
                    out=QTc[:, t * 512:(t + 1) * 512].rearrange("(c p) n -> p c n", p=128), in_=qcTs[sT][:]),
                  r=[("qcTs", sT)], w=["QTc"], dma=("stqc", sT))
                A("pool", lambda e, t=t, sT=sT: e.dma_start(
                    out=KTc[:, t * 512:(t + 1) * 512].rearrange("(c p) n -> p c n", p=128), in_=kcTs[sT][:]),
                  r=[("kcTs", sT)], w=["KTc"], dma=("stkc", sT))
                A("pool", lambda e, t=t, sV=sV: e.dma_start(
                    out=Vc[t * 512:(t + 1) * 512, :].rearrange("(j p) f -> p j f", p=128),
                    in_=vcst[sV][:].rearrange("p j h e -> p j (h e)")),
                  r=[("vcst", sV)], w=["Vc"], dma=("stvc", sV))
        sc.barrier()
        if upto == "P1":
            sc.emit(); es.close(); return nc

        with ExitStack() as st:
            strips = sb("strips", [128, 8, SW], BF16, st)
            cstrip = sb("cstrip", [128, 1024], BF16, st)
            selE = sb("selE", [32, 32 * 128], BF16, st)
            cfar = sb("cfar", [128, 8], F32, st)
            KT = [sb("KT%d" % i, [128, S], BF16, st) for i in range(2)]
            QT = [sb("QT%d" % i, [128, S], BF16, st) for i in range(2)]
            V1 = [sb("V1%d" % i, [128, NT, 129], BF16, st) for i in range(2)]
            MB = sb("MBh", [32, S], BF16, st)
            PT = [sb("PT%d" % i, [128, 512], BF16, st) for i in range(3)]
            Oev = [sb("Oev%d" % i, [128, 4, 129], F32, st) for i in range(2)]
            rcp = [sb("rcp%d" % i, [128, 8], F32, st) for i in range(2)]
            dtl = [sb("dtl%d" % i, [128, 4, 128], F32, st) for i in range(2)]
            dss = sb("dss", [128, 8], F32, st)
            fin = [sb("fin%d" % i, [128, 4, 128], BF16, st) for i in range(2)]
            ast = [sb("ast%d" % i, [128, 512], BF16, st) for i in range(2)]
            lamw = sb("lamw", [128, 256], F32, st)
            wsub = sb("wsub", [128, 128], F32, st)

            with ExitStack() as st2:
                sstage = [sb("sstg%d" % i, [128, SW], F32, st2) for i in range(2)]
                for hb in range(8):
                    s = hb % 2
                    A("sp", lambda e, s=s, hb=hb: e.dma_start(out=sstage[s][:], in_=strips_in[hb, :, :]),
                      w=[("sstg", s)], dma=("sstg", s))
                    A("pool", lambda e, s=s, hb=hb: e.tensor_copy(out=strips[:, hb, :], in_=sstage[s][:]),
                      r=[("sstg", s)], w=["strips"])
                sc.barrier()
            A("sp", lambda e: e.dma_start(out=cstrip[:], in_=cstrip_in[:, :]), w=["cstrip"], dma="c0")
            A("sp", lambda e: e.dma_start(out=selE[:], in_=sel_in[:, :]), w=["selE"], dma="c1")
            bcast_row(cfar[:], relb[31:32, :], "cfar")
            bcast_row(lamw[:], dlam[l:l + 1, :], "lamw")
            bcast_row(wsub[:], dsub[l:l + 1, :], "wsub0")

            def lamf(e):
                e.tensor_tensor(out=lamw[:, 0:64], in0=lamw[:, 0:64], in1=lamw[:, 64:128], op=ALU.mult)
                e.tensor_tensor(out=lamw[:, 128:192], in0=lamw[:, 128:192], in1=lamw[:, 192:256], op=ALU.mult)
                e.tensor_reduce(out=lamt[:, 0:1], in_=lamw[:, 0:64], axis=AX.X, op=ALU.add)
                return e.tensor_reduce(out=lamt[:, 1:2], in_=lamw[:, 128:192], axis=AX.X, op=ALU.add)
            A("dve", lamf, r=["lamw"], w=["lam0"])
            A("act", lambda e: e.activation(out=lamt[:, 0:2], in_=lamt[:, 0:2], func=AF.Exp), r=["lam0"], w=["lam1"])

            def lamg(e):
                e.tensor_tensor(out=lamt[:, 2:3], in0=lamt[:, 1:2], in1=lamt[:, 0:1], op=ALU.subtract)
                e.tensor_scalar(out=lamt[:, 2:3], in0=lamt[:, 2:3], scalar1=float(-lam_init), scalar2=None, op0=ALU.add)
                return e.tensor_scalar(out=wsub[:], in0=wsub[:], scalar1=float(1.0 - lam_init), scalar2=None, op0=ALU.mult)
            A("dve", lamg, r=["lam1", "wsub0"], w=["lam", "wsub"])

            state = {"step": 0, "oset": 0, "head": 0}
            OB = [(PF[3], PF[4]), (PW[:, 0:512], PW[:, 512:1024])]

            def attn_tiles(tiles, hs, dk, dv, bias_kind, hb, mb):
                dv1 = dv + 1
                steps = []
                for ti, (kb, qt, cb) in enumerate(tiles):
                    nk = 4 * (qt + 1)
                    for kc in range(nk):
                        steps.append((ti, kb, qt, kc, kc == nk - 1, cb))
                osets = {}

                def qk(i):
                    ti, kb, qt, kc, lastk, cb = steps[i]
                    g = state["step"] + i
                    b = g % 3
                    dl = qt * 512 - kc * 128
                    near = dl <= 896 if bias_kind == "t5" else dl <= 0

                    def f(e, b=b, kb=kb, qt=qt, kc=kc, dl=dl, near=near):
                        more = near or mb
                        ins = e.matmul(PF[b][:], lhsT=KT[hs][kb:kb + dk, kc * 128:(kc + 1) * 128],
                                       rhs=QT[hs][kb:kb + dk, qt * 512:(qt + 1) * 512], start=True, stop=not more)
                        if near:
                            src = strips[:, hb, dl + 511:dl + 1023] if bias_kind == "t5" else cstrip[:, dl + 511:dl + 1023]
                            ins = e.matmul(PF[b][:], lhsT=identb[:], rhs=src, start=False, stop=not mb)
                        if mb:
                            n = kc // 2
                            ins = e.matmul(PF[b][:], lhsT=selE[:, n * 128:(n + 1) * 128],
                                           rhs=MB[:, qt * 512:(qt + 1) * 512], start=False, stop=True)
                        return ins
                    rr = [("KT", hs), ("QT", hs), "identb", "strips", "cstrip"]
                    if mb:
                        rr += ["selE", "MBh"]
                    A("pe", f, r=rr, w=[("S", b)])
                    return near

                nears = {}
                nears[0] = qk(0)
                if len(steps) > 1:
                    nears[1] = qk(1)
                for i in range(len(steps)):
                    ti, kb, qt, kc, lastk, cb = steps[i]
                    if i + 2 < len(steps):
                        nears[i + 2] = qk(i + 2)
                    g = state["step"] + i
                    b = g % 3
                    near = nears[i]
                    if kc == 0:
                        osets[ti] = state["oset"] % 2
                        state["oset"] += 1
                    os_ = osets[ti]
                    if bias_kind == "t5" and not near:
                        A("act", lambda e, b=b: e.activation(out=PT[b][:], in_=PF[b][:], func=AF.Exp,
                                                              bias=cfar[:, hb:hb + 1]),
                          r=[("S", b), "cfar"], w=[("PT", b)])
                    else:
                        A("act", lambda e, b=b: e.activation(out=PT[b][:], in_=PF[b][:], func=AF.Exp),
                          r=[("S", b)], w=[("PT", b)])

                    def pv(e, b=b, qt=qt, kc=kc, os_=os_):
                        ins = None
                        for j in range(4):
                            if kc > 4 * qt + j:
                                continue
                            ob = OB[os_][j // 2]
                            c0 = (j % 2) * 129
                            ins = e.matmul(ob[:, c0:c0 + dv1], lhsT=PT[b][:, j * 128:(j + 1) * 128],
                                           rhs=V1[hs][:, kc, 0:dv1], start=(kc == 0 and j % 2 == 0),
                                           stop=(kc == 4 * qt + j), skip_group_check=True)
                        return ins
                    A("pe", pv, r=[("PT", b), ("V1", hs)], w=[("O", os_)])
                    if lastk:
                        A("dve", lambda e, os_=os_: (
                            e.tensor_copy(out=Oev[os_][:, 0:2, 0:dv1],
                                          in_=OB[os_][0][:, 0:258].rearrange("p (a c) -> p a c", a=2)[:, :, 0:dv1]),
                            e.tensor_copy(out=Oev[os_][:, 2:4, 0:dv1],
                                          in_=OB[os_][1][:, 0:258].rearrange("p (a c) -> p a c", a=2)[:, :, 0:dv1]))[1],
                          r=[("O", os_)], w=[("Oev", os_)], par=True)
                        cb(os_, qt)
                state["step"] += len(steps)

            def store_fin(fs, nfeat, row0, qt):
                sa = state["head"] % 2
                state["head"] += 1

                def tr(e):
                    for j in range(4):
                        ins = e.transpose(out=PB[0:nfeat, j * 128:(j + 1) * 128], in_=fin[fs][:, j, 0:nfeat],
                                          identity=identb[:])
                    return ins
                A("pe", tr, r=[("fin", fs), "identb"], w=["PB"])
                A("act", lambda e, sa=sa: e.copy(out=ast[sa][0:nfeat, :], in_=PB[0:nfeat, 0:512]),
                  r=["PB"], w=[("ast", sa)])
                A("pool", lambda e, sa=sa: e.dma_start(out=attT[row0:row0 + nfeat, qt * 512:(qt + 1) * 512],
                                                       in_=ast[sa][0:nfeat, :]),
                  r=[("ast", sa)], w=["attT"], dma=("stat", sa))

            def load_head(hs, ktsrc, qtsrc, nrow, vsrc, vc0, dv1, mbsrc=None):
                A("sp", lambda e: e.dma_start(out=KT[hs][0:nrow, :], in_=ktsrc), r=["QTd", "KTd", "QTm", "KTm", "QTc", "KTc"],
                  w=[("KT", hs)], dma=("ldk", hs))
                A("sp", lambda e: e.dma_start(out=QT[hs][0:nrow, :], in_=qtsrc), r=["QTd", "KTd", "QTm", "KTm", "QTc", "KTc"],
                  w=[("QT", hs)], dma=("ldq", hs))
                A("sp", lambda e: e.dma_start(out=V1[hs][:, :, 0:dv1],
                                              in_=vsrc[:, vc0:vc0 + dv1].rearrange("(c p) f -> p c f", p=128)),
                  r=["Vd", "Vm", "Vc"], w=[("V1", hs)], dma=("ldv", hs))
                if mbsrc is not None:
                    A("sp", lambda e: e.dma_start(out=MB[:], in_=mbsrc), r=["MBT"], w=["MBh"], dma="ldmb")

            hcount = 0
            for h in range(4):
                hs = hcount % 2; hcount += 1
                load_head(hs, KTd[h * 128:(h + 1) * 128, :], QTd[h * 128:(h + 1) * 128, :], 128, Vd, h * 129, 129)
                pend = {}

                def cb0(os_, qt):
                    pend[qt] = os_

                def cb1(os_, qt, h=h):
                    o1, o2 = pend[qt], os_
                    fs = qt % 2

                    def comb0(e):
                        e.reciprocal(out=rcp[0][:, 0:4], in_=Oev[o1][:, :, 128])
                        e.reciprocal(out=rcp[0][:, 4:8], in_=Oev[o2][:, :, 128])
                    A("dve", comb0, r=[("Oev", o1), ("Oev", o2)], w=["rcp0"], par=True)
                    A("dve", lambda e: e.tensor_scalar(out=rcp[0][:, 4:8], in0=rcp[0][:, 4:8], scalar1=lamt[:, 2:3],
                                                       scalar2=None, op0=ALU.mult), r=["rcp0", "lam"], w=["rcp0b"])

                    def comb1(e):
                        for j in range(4):
                            e.tensor_scalar(out=dtl[0][:, j, :], in0=Oev[o1][:, j, 0:128], scalar1=rcp[0][:, j:j + 1],
                                            scalar2=None, op0=ALU.mult)
                    A("dve", comb1, r=[("Oev", o1), "rcp0b"], w=["dtl0"], par=True)

                    def comb2(e):
                        for j in range(4):
                            e.scalar_tensor_tensor(out=dtl[0][:, j, :], in0=Oev[o2][:, j, 0:128],
                                                   scalar=rcp[0][:, 4 + j:5 + j], in1=dtl[0][:, j, :],
                                                   op0=ALU.mult, op1=ALU.add)
                    A("dve", comb2, r=[("Oev", o2), "rcp0b", "dtl0"], w=["dtl"], par=True)

                    def sq(e):
                        for j in range(4):
                            e.activation(out=dtl[1][:, j, :], in_=dtl[0][:, j, :], func=AF.Square,
                                         accum_out=dss[:, j:j + 1])
                    A("act", sq, r=["dtl"], w=["dss0"], par=True)
                    A("act", lambda e: rms_rstd(e, dss[:, 4:8], dss[:, 0:4], 128), r=["dss0", "eps"], w=["dss"])

                    def nrm(e):
                        for j in range(4):
                            ins = e.scalar_tensor_tensor(out=fin[fs][:, j, :], in0=dtl[0][:, j, :],
                                                         scalar=dss[:, 4 + j:5 + j], in1=wsub[:],
                                                         op0=ALU.mult, op1=ALU.mult)
                        return ins
                    A("dve", nrm, r=["dtl", "dss", "wsub"], w=[("fin", fs)], par=True)
                    store_fin(fs, 128, h * 128, qt)
                tiles = []
                for qt in range(NQ):
                    tiles.append((0, qt, cb0))
                    tiles.append((64, qt, cb1))
                attn_tiles(tiles, hs, 64, 128, "t5", h, False)

            for h in range(4):
                hs = hcount % 2; hcount += 1
                load_head(hs, KTm[h * 128:(h + 1) * 128, :], QTm[h * 128:(h + 1) * 128, :], 128, Vm, h * 129, 129,
                          MBT[h * 32:(h + 1) * 32, :])

                def cbm(os_, qt, h=h):
                    fs = qt % 2

                    A("dve", lambda e: e.reciprocal(out=rcp[1][:, 0:4], in_=Oev[os_][:, :, 128]), r=[("Oev", os_)], w=["rcp1"])

                    def f(e):
                        for j in range(4):
                            e.tensor_scalar(out=fin[fs][:, j, :], in0=Oev[os_][:, j, 0:128],
                                            scalar1=rcp[1][:, j:j + 1], scalar2=None, op0=ALU.mult)
                    A("dve", f, r=[("Oev", os_), "rcp1"], w=[("fin", fs)], par=True)
                    store_fin(fs, 128, 512 + h * 128, qt)
                attn_tiles([(0, qt, cbm) for qt in range(NQ)], hs, 128, 128, "t5", 4 + h, True)

            for h in range(8):
                hs = hcount % 2; hcount += 1
                load_head(hs, KTc[h * 96:(h + 1) * 96, :], QTc[h * 96:(h + 1) * 96, :], 96, Vc, h * 65, 65)

                def cbc(os_, qt, h=h):
                    fs = qt % 2

                    A("dve", lambda e: e.reciprocal(out=rcp[1][:, 0:4], in_=Oev[os_][:, :, 64]), r=[("Oev", os_)], w=["rcp1"])

                    def f(e):
                        for j in range(4):
                            e.tensor_scalar(out=fin[fs][:, j, 0:64], in0=Oev[os_][:, j, 0:64],
                                            scalar1=rcp[1][:, j:j + 1], scalar2=None, op0=ALU.mult)
                    A("dve", f, r=[("Oev", os_), "rcp1"], w=[("fin", fs)], par=True)
                    store_fin(fs, 64, 1024 + h * 64, qt)
                attn_tiles([(0, qt, cbc) for qt in range(NQ)], hs, 96, 64, "causal", 0, False)
        sc.barrier()
        if upto == "P2":
            sc.emit(); es.close(); return nc

        with ExitStack() as st:
            wg = sb("wg", [128, 8, 3072], BF16, st)
            wbr_b = sb("wbr_b", [128, 12, D], BF16, st)
            wout_b = sb("wout_b", [128, 8, D], BF16, st)
            wpost = sb("wpost", [128, D], F32, st)
            with ExitStack() as st2:
                stage = [sb("stg3_%d" % i, [128, 1536], F32, st2) for i in range(2)]
                load_cast(lambda kc, c0, cw: wg[:, kc, c0:c0 + cw],
                          [w_in[l, kc * 128:(kc + 1) * 128, C_G:DIN] for kc in range(8)], 3072, stage, 2, "wst", "wg", 1536)
                load_cast(lambda kc, c0, cw: wbr_b[:, kc, c0:c0 + cw],
                          [wbr[l, kc * 128:(kc + 1) * 128, :] for kc in range(12)], D, stage, 2, "wst", "wbr", D)
                load_cast(lambda kc, c0, cw: wout_b[:, kc, c0:c0 + cw],
                          [wout[l, kc * 128:(kc + 1) * 128, :] for kc in range(8)], D, stage, 2, "wst", "wout", D)
                bcast_row(wpost[:], n_mpo[l:l + 1, :], "wpost")
                sc.barrier()
            hTt = [sb("hTt3_%d" % i, [128, 8, 512], BF16, st) for i in range(2)]
            aTt = [sb("aTt%d" % i, [128, 12, 512], BF16, st) for i in range(2)]
            xt = [sb("xt3_%d" % i, [128, 4, D], F32, st) for i in range(1)]
            mixT = sb("mixT", [128, 8, 512], BF16, st)
            sg = [sb("sg%d" % i, [128, 512], F32, st) for i in range(3)]
            pr = [sb("pr%d" % i, [128, 512], F32, st) for i in range(3)]
            junk = sb("junk3", [128, D], BF16, st)
            ss = sb("ss3", [128, 2 * NT], F32, st)
            tmp = [sb("tmp3_%d" % i, [128, D], F32, st) for i in range(2)]
            gi = 0
            for t in range(NQ):
                s = t % 2
                A("sp", lambda e, s=s, t=t: e.dma_start(
                    out=hTt[s][:], in_=hT_d[:, t * 512:(t + 1) * 512].rearrange("(c p) n -> p c n", p=128)),
                  r=["hT_d"], w=[("hTt", s)], dma=("ldh", s))
                A("sp", lambda e, s=s, t=t: e.dma_start(
                    out=aTt[s][:], in_=attT[:, t * 512:(t + 1) * 512].rearrange("(c p) n -> p c n", p=128)),
                  r=["attT"], w=[("aTt", s)], dma=("lda", s))
                A("sp", lambda e, s=s, t=t: e.dma_start(
                    out=xt[0][:], in_=x_cur[t * 512:(t + 1) * 512, :].rearrange("(j p) d -> p j d", p=128)),
                  r=["xa", "xb"], w=[("xt", 0)], dma=("ldx", 0))
                for oc in range(8):
                    for g in range(3):
                        bg = gi % 6
                        bb = (gi + 1) % 6
                        k3 = (gi // 2) % 3
                        gi += 2

                        def mg(e, bg=bg, g=g, oc=oc):
                            for kc in range(8):
                                ins = e.matmul(PF[bg][:], lhsT=wg[:, kc, g * D + oc * 128:g * D + (oc + 1) * 128],
                                               rhs=hTt[s][:, kc, :], start=(kc == 0), stop=(kc == 7))
                            return ins
                        A("pe", mg, r=["wg", ("hTt", s)], w=[("PF", bg)])

                        def mb_(e, bb=bb, g=g, oc=oc):
                            for c in range(4):
                                ins = e.matmul(PF[bb][:], lhsT=wbr_b[:, g * 4 + c, oc * 128:(oc + 1) * 128],
                                               rhs=aTt[s][:, g * 4 + c, :], start=(c == 0), stop=(c == 3))
                            return ins
                        A("pe", mb_, r=["wbr", ("aTt", s)], w=[("PF", bb)])
                        A("act", lambda e, bg=bg, k3=k3: e.activation(out=sg[k3][:], in_=PF[bg][:], func=AF.Sigmoid),
                          r=[("PF", bg)], w=[("sg", k3)])
                        A("dve", lambda e, bb=bb, k3=k3, g=g: e.tensor_tensor(out=pr[g][:], in0=sg[k3][:], in1=PF[bb][:],
                                                                               op=ALU.mult),
                          r=[("sg", k3), ("PF", bb)], w=[("pr", g)])
                    A("pool", lambda e: e.tensor_tensor(out=pr[0][:], in0=pr[0][:], in1=pr[1][:], op=ALU.add),
                      r=[("pr", 0), ("pr", 1)], w=[("pr", 0)])
                    A("pool", lambda e, oc=oc: e.tensor_tensor(out=mixT[:, oc, :], in0=pr[0][:], in1=pr[2][:], op=ALU.add),
                      r=[("pr", 0), ("pr", 2)], w=[("mixT", oc)])
                for j in range(4):
                    tj = t * 4 + j
                    s2 = tj % 2

                    def mo(e, j=j):
                        for hf in range(2):
                            for kc in range(8):
                                ins = e.matmul(PW[:, hf * 512:(hf + 1) * 512], lhsT=mixT[:, kc, j * 128:(j + 1) * 128],
                                               rhs=wout_b[:, kc, hf * 512:(hf + 1) * 512], start=(kc == 0), stop=(kc == 7))
                        return ins
                    A("pe", mo, r=[("mixT", oc) for oc in range(8)] + ["wout"], w=["PW"])
                    A("act", lambda e, tj=tj: (e.activation(out=junk[:], in_=PW[:], func=AF.Square,
                                                            accum_out=ss[:, 2 * tj:2 * tj + 1]),
                                               rms_rstd(e, ss[:, 2 * tj + 1:2 * tj + 2], ss[:, 2 * tj:2 * tj + 1], D)),
                      r=["PW", "eps"], w=[("ss", tj)])

                    def fz(e, s=s, j=j, tj=tj, s2=s2):
                        return e.scalar_tensor_tensor(out=tmp[s2][:], in0=PW[:], scalar=ss[:, 2 * tj + 1:2 * tj + 2],
                                                      in1=wpost[:], op0=ALU.mult, op1=ALU.mult)
                    A("dve", fz, r=["PW", ("ss", tj), "wpost"], w=[("tmp", s2)])
                    A("pool", lambda e, s=s, j=j, s2=s2: e.tensor_tensor(out=xt[0][:, j, :], in0=xt[0][:, j, :], in1=tmp[s2][:],
                                                                          op=ALU.add),
                      r=[("tmp", s2), ("xt", 0)], w=[("xt", 0)])
                A("pool", lambda e, s=s, t=t: e.dma_start(
                    out=xa[t * 512:(t + 1) * 512, :].rearrange("(j p) d -> p j d", p=128), in_=xt[0][:]),
                  r=[("xt", 0)], w=["xa"], dma=("stx", 0))
        sc.barrier()
        if upto == "P3":
            sc.emit(); es.close(); return nc

        with ExitStack() as st:
            wup_b = sb("wup_b", [128, 8, DFF], BF16, st)
            wdn_b = sb("wdn_b", [128, 32, D], BF16, st)
            wpre = sb("wpre4", [128, D], F32, st)
            wpost = sb("wpost4", [128, D], F32, st)
            with ExitStack() as st2:
                stage = [sb("stg4_%d" % i, [128, 2048], F32, st2) for i in range(2)]
                load_cast(lambda kc, c0, cw: wup_b[:, kc, c0:c0 + cw],
                          [wup[l, kc * 128:(kc + 1) * 128, :] for kc in range(8)], DFF, stage, 2, "wst", "wup", 2048)
                load_cast(lambda kc, c0, cw: wdn_b[:, kc, c0:c0 + cw],
                          [wdn[l, kc * 128:(kc + 1) * 128, :] for kc in range(32)], D, stage, 2, "wst", "wdn", D)
                bcast_row(wpre[:], n_lp[l:l + 1, :], "wpre4")
                bcast_row(wpost[:], n_lpo[l:l + 1, :], "wpost4")
                sc.barrier()
            xt = [sb("xt4_%d" % i, [128, 2, D], F32, st) for i in range(1)]
            hn = [sb("hn4_%d" % i, [128, D], BF16, st) for i in range(2)]
            h2T = sb("h2T", [128, 8, 256], BF16, st)
            uT = sb("uT", [128, 32, 256], BF16, st)
            rl = [sb("rl%d" % i, [128, 256], F32, st) for i in range(2)]
            ss = sb("ss4", [128, 4 * NT], F32, st)
            tmp = [sb("tmp4_%d" % i, [128, D], F32, st) for i in range(1)]
            ui = 0
            for t in range(S // 256):
                s = t % 2
                A("sp", lambda e, s=s, t=t: e.dma_start(
                    out=xt[0][:], in_=xa[t * 256:(t + 1) * 256, :].rearrange("(j p) d -> p j d", p=128)),
                  r=["xa"], w=[("xt", 0)], dma=("ldx", 0))
                for j in range(2):
                    tj = t * 2 + j
                    s2 = tj % 2
                    A("act", lambda e, s=s, j=j, tj=tj: e.activation(out=hn[tj % 2][:], in_=xt[0][:, j, :], func=AF.Square,
                                                                       accum_out=ss[:, 4 * tj:4 * tj + 1]),
                      r=[("xt", 0)], w=[("ssa0", tj), ("hn", tj % 2)])
                    A("act", lambda e, tj=tj: rms_rstd(e, ss[:, 4 * tj + 1:4 * tj + 2], ss[:, 4 * tj:4 * tj + 1], D),
                      r=[("ssa0", tj), "eps"], w=[("ssa", tj)])

                    def nf(e, s=s, j=j, tj=tj, s2=s2):
                        return e.scalar_tensor_tensor(out=hn[s2][:], in0=xt[0][:, j, :], scalar=ss[:, 4 * tj + 1:4 * tj + 2],
                                                      in1=wpre[:], op0=ALU.mult, op1=ALU.mult)
                    A("dve", nf, r=[("xt", 0), ("ssa", tj), "wpre4"], w=[("hn", s2)])

                    def tr8(e, s2=s2):
                        for c in range(8):
                            ins = e.transpose(out=PB[:, c * 128:(c + 1) * 128], in_=hn[s2][:, c * 128:(c + 1) * 128],
                                              identity=identb[:])
                        return ins
                    A("pe", tr8, r=[("hn", s2), "identb"], w=["PB"])
                    A("act", lambda e, j=j: e.copy(out=h2T[:, :, j * 128:(j + 1) * 128],
                                                   in_=PB.rearrange("p (c n) -> p c n", c=8)),
                      r=["PB"], w=["h2T"])
                for fc in range(32):
                    b = ui % 5
                    k2 = ui % 2
                    ui += 1

                    def mu(e, b=b, fc=fc):
                        for kc in range(8):
                            ins = e.matmul(PF[b][:, 0:256], lhsT=wup_b[:, kc, fc * 128:(fc + 1) * 128], rhs=h2T[:, kc, :],
                                           start=(kc == 0), stop=(kc == 7))
                        return ins
                    A("pe", mu, r=["wup", "h2T"], w=[("PF", b)])
                    A("act", lambda e, b=b, k2=k2: e.activation(out=rl[k2][:], in_=PF[b][:, 0:256], func=AF.Relu),
                      r=[("PF", b)], w=[("rl", k2)])
                    A("dve", lambda e, k2=k2, fc=fc: e.tensor_tensor(out=uT[:, fc, :], in0=rl[k2][:], in1=rl[k2][:], op=ALU.mult),
                      r=[("rl", k2)], w=[("uT", fc)])
                for j in range(2):
                    tj = t * 2 + j
                    s2 = tj % 2

                    def md(e, j=j):
                        for hf in range(2):
                            for fc in range(32):
                                ins = e.matmul(PW[:, hf * 512:(hf + 1) * 512], lhsT=uT[:, fc, j * 128:(j + 1) * 128],
                                               rhs=wdn_b[:, fc, hf * 512:(hf + 1) * 512], start=(fc == 0), stop=(fc == 31))
                        return ins
                    A("pe", md, r=[("uT", fc) for fc in range(32)] + ["wdn"], w=["PW"])
                    A("act", lambda e, tj=tj: e.activation(out=tmp[0][:], in_=PW[:], func=AF.Square,
                                                           accum_out=ss[:, 4 * tj + 2:4 * tj + 3]),
                      r=["PW"], w=[("ssb0", tj), ("tmp", 0)])
                    A("act", lambda e, tj=tj: rms_rstd(e, ss[:, 4 * tj + 3:4 * tj + 4], ss[:, 4 * tj + 2:4 * tj + 3], D),
                      r=[("ssb0", tj), "eps"], w=[("ssb", tj)])

                    def fz(e, tj=tj, s2=s2):
                        return e.scalar_tensor_tensor(out=tmp[0][:], in0=PW[:], scalar=ss[:, 4 * tj + 3:4 * tj + 4],
                                                      in1=wpost[:], op0=ALU.mult, op1=ALU.mult)
                    A("dve", fz, r=["PW", ("ssb", tj), "wpost4"], w=[("tmp", 0)])
                    A("pool", lambda e, s=s, j=j, s2=s2: e.tensor_tensor(out=xt[0][:, j, :], in0=xt[0][:, j, :], in1=tmp[0][:],
                                                                          op=ALU.add),
                      r=[("tmp", 0), ("xt", 0)], w=[("xt", 0)])
                A("pool", lambda e, s=s, t=t: e.dma_start(
                    out=x_fin[t * 256:(t + 1) * 256, :].rearrange("(j p) d -> p j d", p=128), in_=xt[0][:]),
                  r=[("xt", 0)], w=["xb"], dma=("stx", 0))
        sc.barrier()
        x_cur = xb

    sc.emit()
    es.close()
    return nc


def _t5_bucket_np(rel):
    n = np.maximum(rel, 0)
    nf = np.maximum(n, 1).astype(np.float32)
    large = 16 + (np.log(nf / np.float32(16)) / np.float32(math.log(64)) * np.float32(16)).astype(np.int32)
    large = np.minimum(large, 31)
    return np.where(n < 16, n, large)


def host_consts(rel_bias):
    kk = np.arange(128)[:, None]
    c = np.arange(SW)[None, :]
    rel = c - kk - 511
    bidx = _t5_bucket_np(rel)
    rb = np.asarray(rel_bias, np.float32)
    strips = np.empty((8, 128, SW), np.float32)
    for h in range(8):
        g = rb[bidx, h]
        strips[h] = np.where(rel >= 0, g, np.float32(NEG))
    c2 = np.arange(1024)[None, :]
    cstrip = np.where(c2 - kk - 511 >= 0, 0.0, NEG).astype(ml_dtypes.bfloat16)
    identb = np.eye(128, dtype=np.float32).astype(ml_dtypes.bfloat16)
    identf = np.eye(128, dtype=np.float32)
    sel = np.zeros((32, 32 * 128), np.float32)
    for n in range(32):
        sel[n, n * 128:(n + 1) * 128] = 1.0
    return dict(strips=strips, cstrip=cstrip, identb=identb, identf=identf, selE=sel.astype(ml_dtypes.bfloat16))


def make_in_maps(x, positions, rel_bias, norm_mix_pre, norm_mix_post, norm_mlp_pre, norm_mlp_post,
                 w_in, diff_lambda, diff_subln, mla_q_norm, mla_w_uq, mla_kv_norm, mla_w_ukv,
                 w_branch, w_out, w_up, w_down, n_cores=8):
    B, S, _ = x.shape
    L = w_in.shape[0]
    f = lambda a: np.ascontiguousarray(np.asarray(a, np.float32))
    shared = dict(
        relb=f(rel_bias), n_mp=f(norm_mix_pre), n_mpo=f(norm_mix_post), n_lp=f(norm_mlp_pre), n_lpo=f(norm_mlp_post),
        w_in=f(w_in), dlam=f(diff_lambda).reshape(L, 256), dsub=f(diff_subln), mqn=f(mla_q_norm), wuq=f(mla_w_uq),
        mkn=f(mla_kv_norm), wukv=f(mla_w_ukv), wbr=f(w_branch).reshape(L, 1536, D), wout=f(w_out), wup=f(w_up),
        wdn=f(w_down))
    shared.update(host_consts(rel_bias))
    maps = []
    for c in range(n_cores):
        b = c % B
        m = dict(shared)
        m["x"] = f(x[b])
        m["pos"] = np.ascontiguousarray(np.asarray(positions[b], np.int32).reshape(S // 128, 128).T)
        maps.append(m)
    return maps


_NC_CACHE = {}


def kernel(**inputs):
    x = np.asarray(inputs["x"])
    B, S, _ = x.shape
    L = np.asarray(inputs["w_in"]).shape[0]
    key = (S, L)
    if key not in _NC_CACHE:
        _NC_CACHE[key] = build(S, L)
    nc = _NC_CACHE[key]
    maps = make_in_maps(**inputs, n_cores=B)
    res = run_bass_kernel_spmd(nc, maps, core_ids=list(range(B)))
    out = np.stack([np.asarray(res.results[b]["y"], np.float32) for b in range(B)], axis=0)
    return out
```
